# Optimizing a Trainium2 kernel written in Bass

```python
import jax, jax.numpy as jnp
from jax import lax
import numpy as np

D_MODEL = 4096
BATCH = 2
SEQ = 8192
DEPTH = 1

D_MIX = D_MODEL
A_WIDTH = D_MIX // 2
A_HEADS = 8
A_HEAD_DIM = A_WIDTH // A_HEADS
A_CHUNK = 128
B_WIDTH = D_MIX - A_WIDTH
B_HEADS = 4
B_KEY_WIDTH = B_WIDTH // 2
B_DK = B_KEY_WIDTH // B_HEADS
B_DV = B_WIDTH // B_HEADS
B_GATE_RANK = 16
B_GATE_TAU = 16.0
B_CHUNK = 64
IN_WIDTHS = (A_WIDTH, A_WIDTH, B_KEY_WIDTH, B_KEY_WIDTH, B_WIDTH, B_WIDTH, B_GATE_RANK)
D_IN = A_WIDTH * 2 + B_KEY_WIDTH * 2 + B_WIDTH * 2 + B_GATE_RANK
IN_SPLITS = (A_WIDTH, 2 * A_WIDTH, 2 * A_WIDTH + B_KEY_WIDTH, 2 * A_WIDTH + 2 * B_KEY_WIDTH,
             2 * A_WIDTH + 2 * B_KEY_WIDTH + B_WIDTH, 2 * A_WIDTH + 2 * B_KEY_WIDTH + 2 * B_WIDTH)
N_EXPERTS = 64
TOP_K = 6
N_GROUPS = 8
TOPK_GROUPS = 4
D_EXPERT = 768
ROUTED_SCALE = 2.5
MOE_BLOCK = 128
N_MOD = 6
EPS = 1e-6

kernel_name = "hybrid_sgu_gla_moe_adaln_block"


def rms_norm(x, g):
    xf = x.astype(jnp.float32)
    y = xf * lax.rsqrt(jnp.mean(xf * xf, axis=-1, keepdims=True) + EPS)
    return (y * g.astype(jnp.float32)).astype(x.dtype)


def layer_norm(x, g, b):
    xf = x.astype(jnp.float32)
    mu = jnp.mean(xf, axis=-1, keepdims=True)
    var = jnp.mean(jnp.square(xf - mu), axis=-1, keepdims=True)
    return ((xf - mu) * lax.rsqrt(var + EPS) * g.astype(jnp.float32) + b.astype(jnp.float32)).astype(x.dtype)


def chunked_spatial_gating(u, v, ln_g, ln_b, w_s, b_s, out_g):
    bsz, seq, _ = u.shape
    n = seq // A_CHUNK
    v = layer_norm(v, ln_g, ln_b).reshape(bsz, n, A_CHUNK, A_HEADS, A_HEAD_DIM)
    causal = jnp.tril(jnp.ones((A_CHUNK, A_CHUNK), dtype=bool))
    w = jnp.where(causal[None], w_s, 0.0).astype(v.dtype)
    s = jnp.einsum('hij,bnjhc->bnihc', w, v) + b_s.T[None, None, :, :, None].astype(v.dtype)
    y = u * s.reshape(bsz, seq, A_WIDTH)
    return rms_norm(y, out_g)


def gated_linear_attention(q, k, v, r, g_low, w_g2, b_g2, head_g):
    bsz, seq, _ = q.shape
    n = seq // B_CHUNK

    def heads(t, d):
        return t.reshape(bsz, n, B_CHUNK, B_HEADS, d).transpose(0, 3, 1, 2, 4).astype(jnp.float32)

    gate_logits = (g_low @ w_g2 + b_g2).astype(jnp.float32)
    log_a = jax.nn.log_sigmoid(gate_logits) / B_GATE_TAU
    qh = heads(q, B_DK) * (B_DK ** -0.5)
    kh = heads(k, B_DK)
    vh = heads(v, B_DV)
    cum = jnp.cumsum(heads(log_a, B_DK), axis=3)
    last = cum[:, :, :, -1:, :]
    q_dec = qh * jnp.exp(cum)
    k_dec = kh * jnp.exp(-cum)
    k_state = kh * jnp.exp(last - cum)
    causal = jnp.tril(jnp.ones((B_CHUNK, B_CHUNK), dtype=bool))
    attn = jnp.where(causal, jnp.einsum('bhnid,bhnjd->bhnij', q_dec, k_dec), 0.0)
    o_intra = jnp.einsum('bhnij,bhnjv->bhniv', attn, vh)
    decay = jnp.exp(last[:, :, :, 0, :])

    def step(state, xs):
        q_c, k_c, v_c, d_c = xs
        o = jnp.einsum('bhcd,bhdv->bhcv', q_c, state)
        state = state * d_c[..., None] + jnp.einsum('bhcd,bhcv->bhdv', k_c, v_c)
        return state, o

    state0 = jnp.zeros((bsz, B_HEADS, B_DK, B_DV), jnp.float32)
    mv = lambda t: jnp.moveaxis(t, 2, 0)
    _, o_inter = lax.scan(step, state0, (mv(q_dec), mv(k_state), mv(vh), mv(decay)))
    o = o_intra + jnp.moveaxis(o_inter, 0, 2)
    o = rms_norm(o.transpose(0, 2, 3, 1, 4), head_g).reshape(bsz, seq, B_WIDTH)
    return (o * jax.nn.silu(r.astype(jnp.float32))).astype(q.dtype)


def swiglu(x, w_gate, w_up, w_down):
    return (jax.nn.silu(x @ w_gate) * (x @ w_up)) @ w_down


def route(xf, w_router, router_bias):
    t = xf.shape[0]
    scores = jax.nn.sigmoid(xf.astype(jnp.float32) @ w_router.astype(jnp.float32))
    sel = scores + router_bias.astype(jnp.float32)
    grp = sel.reshape(t, N_GROUPS, N_EXPERTS // N_GROUPS)
    grp_score = jnp.sum(lax.top_k(grp, 2)[0], axis=-1)
    _, grp_idx = lax.top_k(grp_score, TOPK_GROUPS)
    grp_mask = jnp.any(grp_idx[:, :, None] == jnp.arange(N_GROUPS)[None, None, :], axis=1)
    expert_mask = jnp.repeat(grp_mask, N_EXPERTS // N_GROUPS, axis=1)
    _, idx = lax.top_k(jnp.where(expert_mask, sel, -jnp.inf), TOP_K)
    w = jnp.take_along_axis(scores, idx, axis=1)
    w = w / jnp.sum(w, axis=-1, keepdims=True) * ROUTED_SCALE
    return idx, w


def routed_experts(xf, idx, wts, w_gate, w_up, w_down):
    t = xf.shape[0]
    n_assign = t * TOP_K
    flat_e = idx.reshape(-1)
    flat_tok = jnp.repeat(jnp.arange(t, dtype=jnp.int32), TOP_K)
    flat_w = wts.reshape(-1)
    order = jnp.argsort(flat_e)
    se = flat_e[order]
    counts = jnp.bincount(flat_e, length=N_EXPERTS)
    padded = (counts + MOE_BLOCK - 1) // MOE_BLOCK * MOE_BLOCK
    start = jnp.cumsum(counts) - counts
    pend = jnp.cumsum(padded)
    pstart = pend - padded
    dest = pstart[se] + jnp.arange(n_assign) - start[se]
    n_blocks = -(-n_assign // MOE_BLOCK) + N_EXPERTS
    n_slots = n_blocks * MOE_BLOCK
    slot_tok = jnp.zeros((n_slots,), jnp.int32).at[dest].set(flat_tok[order])
    slot_w = jnp.zeros((n_slots,), jnp.float32).at[dest].set(flat_w[order])
    block_e = jnp.minimum(jnp.searchsorted(pend, jnp.arange(n_blocks) * MOE_BLOCK, side='right'),
                          N_EXPERTS - 1)

    def step(acc, blk):
        toks, w, e = blk
        yb = swiglu(xf[toks], w_gate[e], w_up[e], w_down[e])
        return acc.at[toks].add(yb.astype(jnp.float32) * w[:, None]), None

    acc0 = jnp.zeros((t, xf.shape[1]), jnp.float32)
    out, _ = lax.scan(step, acc0, (slot_tok.reshape(n_blocks, MOE_BLOCK),
                                   slot_w.reshape(n_blocks, MOE_BLOCK), block_e))
    return out.astype(xf.dtype)


def setup_inputs(seed: int = 0) -> dict:
    key = jax.random.key(seed)
    ks = jax.random.split(key, 26)
    L, D, f32 = DEPTH, D_MODEL, jnp.float32
    nrm = lambda k, shape, s: jax.random.normal(k, shape, f32) * s
    gain = lambda k, n: 1.0 + nrm(k, (L, n), 0.02)
    return {
        "x": nrm(ks[0], (BATCH, SEQ, D), 1.0),
        "c": nrm(ks[1], (BATCH, D), 1.0),
        "w_ada": nrm(ks[2], (L, D, N_MOD * D), 0.2 * D ** -0.5),
        "b_ada": nrm(ks[3], (L, N_MOD * D), 0.01),
        "g_pre_mix": gain(ks[4], D),
        "g_post_mix": gain(ks[5], D),
        "g_pre_ffn": gain(ks[6], D),
        "g_post_ffn": gain(ks[7], D),
        "w_in": nrm(ks[8], (L, D, D_IN), D ** -0.5),
        "a_ln_g": gain(ks[9], A_WIDTH),
        "a_ln_b": nrm(ks[10], (L, A_WIDTH), 0.01),
        "a_w_s": nrm(ks[11], (L, A_HEADS, A_CHUNK, A_CHUNK), A_CHUNK ** -0.5),
        "a_b_s": 1.0 + nrm(ks[12], (L, A_HEADS, A_CHUNK), 0.02),
        "a_out_g": gain(ks[13], A_WIDTH),
        "b_w_g2": nrm(ks[14], (L, B_GATE_RANK, B_KEY_WIDTH), B_GATE_RANK ** -0.5),
        "b_b_g2": nrm(ks[15], (L, B_KEY_WIDTH), 0.01),
        "b_head_g": gain(ks[16], B_DV),
        "w_out": nrm(ks[17], (L, D_MIX, D), D_MIX ** -0.5),
        "w_router": nrm(ks[18], (L, D, N_EXPERTS), D ** -0.5),
        "router_bias": nrm(ks[19], (L, N_EXPERTS), 0.01),
        "we_gate": nrm(ks[20], (L, N_EXPERTS, D, D_EXPERT), D ** -0.5),
        "we_up": nrm(ks[21], (L, N_EXPERTS, D, D_EXPERT), D ** -0.5),
        "we_down": nrm(ks[22], (L, N_EXPERTS, D_EXPERT, D), D_EXPERT ** -0.5),
        "ws_gate": nrm(ks[23], (L, D, D_EXPERT), D ** -0.5),
        "ws_up": nrm(ks[24], (L, D, D_EXPERT), D ** -0.5),
        "ws_down": nrm(ks[25], (L, D_EXPERT, D), D_EXPERT ** -0.5),
    }


def reference(x, c, w_ada, b_ada, g_pre_mix, g_post_mix, g_pre_ffn, g_post_ffn, w_in,
              a_ln_g, a_ln_b, a_w_s, a_b_s, a_out_g, b_w_g2, b_b_g2, b_head_g, w_out,
              w_router, router_bias, we_gate, we_up, we_down, ws_gate, ws_up, ws_down):
    bsz, seq, d = x.shape
    c_act = jax.nn.silu(c)
    for l in range(DEPTH):
        mod = (c_act @ w_ada[l] + b_ada[l])[:, None, :]
        shift_m, scale_m, gate_m, shift_f, scale_f, gate_f = jnp.split(mod, N_MOD, axis=-1)

        h = rms_norm(x, g_pre_mix[l]) * (1.0 + scale_m) + shift_m
        proj = h @ w_in[l]
        u_a, v_a, q_b, k_b, v_b, r_b, g_b = jnp.split(proj, IN_SPLITS, axis=-1)
        y_a = chunked_spatial_gating(u_a, v_a, a_ln_g[l], a_ln_b[l], a_w_s[l], a_b_s[l], a_out_g[l])
        y_b = gated_linear_attention(q_b, k_b, v_b, r_b, g_b, b_w_g2[l], b_b_g2[l], b_head_g[l])
        y = jnp.concatenate([y_a, y_b.astype(y_a.dtype)], axis=-1) @ w_out[l]
        x = x + gate_m * rms_norm(y, g_post_mix[l])

        h = rms_norm(x, g_pre_ffn[l]) * (1.0 + scale_f) + shift_f
        hf = h.reshape(bsz * seq, d)
        idx, wts = route(hf, w_router[l], router_bias[l])
        y = routed_experts(hf, idx, wts, we_gate[l], we_up[l], we_down[l]) \
            + swiglu(hf, ws_gate[l], ws_up[l], ws_down[l])
        x = x + gate_f * rms_norm(y.reshape(bsz, seq, d), g_post_ffn[l])
    return x
```

```python
import os
import numpy as np
import concourse.bass as bass
import concourse.mybir as mybir
from concourse.bass_utils import run_bass_kernel_spmd
from contextlib import ExitStack

F32 = mybir.dt.float32
BF16 = mybir.dt.bfloat16
I32 = mybir.dt.int32
ALU = mybir.AluOpType
AF = mybir.ActivationFunctionType
AX = mybir.AxisListType

D = 4096
NK = D // 128
DIN = 10256
NMOD = 6
AW = 2048
BKW = 1024
BW = 2048
NE = 64
DE = 768
NF = DE // 128
EPS = 1e-6
CAP = 384
NCORES = 8


class Buf:
    __slots__ = ("name", "W", "R", "nd")

    def __init__(self, name):
        self.name = name
        self.W = []
        self.R = []
        self.nd = 0


class Op:
    __slots__ = ("eng", "fn", "deps", "dma", "key", "ticket", "signal", "hard", "raw")


class Sched:
    ENGS = ("pe", "act", "dve", "pool", "sp")

    def __init__(self, nc):
        self.nc = nc
        self.ops = {e: [] for e in self.ENGS}
        self.nops = 0
        self.bar = {e: set() for e in self.ENGS}
        self.dmas_since = []
        self.mute = False

    def barrier(self):
        deps = set(self.dmas_since)
        for e in self.ENGS:
            for o in reversed(self.ops[e]):
                if not o.dma:
                    deps.add(o)
                    break
        for e in self.ENGS:
            self.bar[e] |= deps
        self.dmas_since = []

    def op(self, eng, fn, reads=(), writes=(), dma=False, hard=False, waw=False):
        if self.mute:
            return None
        o = Op()
        o.hard = hard
        o.eng = eng
        o.fn = fn
        o.dma = dma
        o.key = None
        o.ticket = None
        o.signal = False
        deps = set()
        for b in reads:
            for w in b.W:
                deps.add(w)
        o.raw = set(d for d in deps if (not d.dma) and d.eng == eng and eng != "pe")
        for b in writes:
            for r in b.R:
                deps.add(r)
            for w in b.W:
                if waw or not (dma and w.dma):
                    deps.add(w)
        if self.bar[eng]:
            deps |= self.bar[eng]
            self.bar[eng] = set()
        deps.discard(o)
        o.deps = deps
        for b in reads:
            b.R.append(o)
        for b in writes:
            if b.R:
                b.R = []
                b.W = [o]
            else:
                if b.W and (not dma) and (not b.W[-1].dma) and b.W[-1].eng == eng:
                    b.W[-1] = o
                else:
                    b.W.append(o)
        if dma:
            assert len(writes) >= 1
            o.key = writes[0]
            o.key.nd += 1
            o.ticket = 16 * o.key.nd
            self.dmas_since.append(o)
        self.ops[eng].append(o)
        self.nops += 1
        return o

    def emit(self, final_bufs=()):
        nc = self.nc
        for e in self.ENGS:
            for o in self.ops[e]:
                for d in o.deps:
                    if not d.dma and (d.eng != o.eng or o.hard or d in o.raw):
                        d.signal = True
        for e in self.ENGS:
            n = 0
            for o in self.ops[e]:
                if not o.dma and o.signal:
                    n += 1
                    o.ticket = n
        keys = []
        seen = set()
        for e in self.ENGS:
            for o in self.ops[e]:
                if o.dma and id(o.key) not in seen:
                    seen.add(id(o.key))
                    keys.append(o.key)
        with ExitStack() as st:
            esem = {e: st.enter_context(nc.semaphore("sem_" + e)) for e in self.ENGS}
            ksem = {id(k): st.enter_context(nc.semaphore("sd_%d" % i)) for i, k in enumerate(keys)}
            self.nsem = len(keys) + len(esem)
            block = st.enter_context(nc.Block())
            engobj = {"pe": block.tensor, "act": block.scalar, "dve": block.vector,
                      "pool": block.gpsimd, "sp": block.sync}

            def make(ename):
                ops = self.ops[ename]

                def body(eng):
                    waited = {}
                    for o in ops:
                        need = {}
                        for d in o.deps:
                            if d.dma:
                                s = ksem[id(d.key)]
                                sk = ("k", id(d.key))
                            else:
                                if d.eng == ename and not (o.hard or d in o.raw):
                                    continue
                                s = esem[d.eng]
                                sk = ("e", d.eng)
                            if need.get(sk, (None, 0))[1] < d.ticket:
                                need[sk] = (s, d.ticket)
                        for sk, (s, v) in need.items():
                            if waited.get(sk, 0) < v:
                                eng.wait_ge(s, v)
                                waited[sk] = v
                        ins = o.fn(eng)
                        if o.dma:
                            ins.then_inc(ksem[id(o.key)], 16)
                        elif o.signal:
                            ins.then_inc(esem[ename], 1)
                    if ename == "sp":
                        for b in final_bufs:
                            if b.nd:
                                eng.wait_ge(ksem[id(b)], 16 * b.nd)
                return body

            for ename in self.ENGS:
                engobj[ename](make(ename))


def build(nt=16, phases=99, debug=False):
    T = nt * 128
    G = min(8, nt)
    NG = nt // G
    GT = G * 128
    nc = bass.Bass("TRN2", target_bir_lowering=False)
    S = Sched(nc)

    def din(name, shape, dt=F32):
        return nc.dram_tensor(name, list(shape), dt, kind="ExternalInput").ap()

    def dscr(name, shape, dt=F32, out=False):
        return nc.dram_tensor(name, list(shape), dt,
                              kind="ExternalOutput" if (out or debug) else "Internal").ap()

    INSH = {
        "x": [T, D], "cT": [128, NK], "w_ada": [D, NMOD * D], "b_ada": [1, NMOD * D],
        "gT_pre_mix": [128, NK], "gT_pre_ffn": [128, NK], "g_post_mix": [1, D], "g_post_ffn": [1, D],
        "w_in": [D, DIN], "a_ln_g": [1, AW], "a_ln_b": [1, AW], "a_w_s": [8, 128, 128],
        "a_b_sT": [128, 8], "a_out_g": [1, AW], "b_w_g2a": [17, BKW], "b_head_g": [1, 512],
        "w_out": [D, D], "w_router": [D, NE], "router_bias": [1, NE],
        "we_gate": [NE, D, DE], "we_up": [NE, D, DE], "we_down": [NE, DE, D],
        "ws_gate": [D, DE], "ws_up": [D, DE], "ws_down": [DE, D],
        "xprev": [3 * T, D], "pmask": [128, 3 * nt], "g_pre_ffn": [1, D],
    }
    used_inputs = {}

    def I(name):
        if name not in used_inputs:
            used_inputs[name] = nc.dram_tensor(name, list(INSH[name]), F32, kind="ExternalInput").ap()
        return used_inputs[name]

    out = dscr("out", [T, D], out=True)

    modrow = dscr("modrow", [1, NMOD * D])
    pu = dscr("p_u", [T, AW])
    pva = dscr("p_va", [T, AW])
    pq = dscr("p_q", [T, BKW])
    pk = dscr("p_k", [T, BKW])
    pvb = dscr("p_vb", [T, BW])
    pr = dscr("p_r", [T, BW])
    pg = dscr("p_g", [T, 16])
    NP = 3 * nt
    ppk = dscr("pp_k", [NP * 128, BKW])
    ppv = dscr("pp_vb", [NP * 128, BW])
    ppg = dscr("pp_g", [NP * 128, 16])
    ycat = dscr("ycat", [T, D], BF16)
    yraw = dscr("yraw", [T, D])
    x1 = dscr("x1", [T, D])
    xn2 = dscr("xn2", [T, D], BF16)
    ysh = dscr("ysh", [T, D])

    B = Buf
    b_modrow = B("modrow")

    def dma(eng, out_ap, in_ap, reads, writes):
        return S.op(eng, lambda e: e.dma_start(out=out_ap, in_=in_ap), reads, writes, dma=True)

    dbg_list = []
    bg_tasks = []
    regcache = {}

    def dbg(name, ap, buf, shape, dt=F32):
        if not debug:
            return
        t_ = nc.dram_tensor("dbg_" + name, list(shape), dt, kind="ExternalOutput").ap()
        if not dbg_list:
            dbg_list.append(B("dbg_all"))
        dma("sp", t_, ap, (buf,), (dbg_list[0],))

    with ExitStack() as top:
        sb = lambda name, shape, dt=F32: top.enter_context(nc.sbuf_tensor(name, list(shape), dt))
        ident_bf = sb("ident_bf", [128, 128], BF16)
        ident_f = sb("ident_f", [128, 128], F32)
        b_ident = B("ident")
        S.op("pool", lambda e: e.memset(ident_f[:, :], 0.0), (), (b_ident,))
        S.op("pool", lambda e: e.affine_select(out=ident_f[:, :], in_=ident_f[:, :], pattern=[[-1, 128]],
                                               compare_op=ALU.not_equal, fill=1.0, base=0,
                                               channel_multiplier=1), (b_ident,), (b_ident,))
        S.op("pool", lambda e: e.tensor_copy(out=ident_bf[:, :], in_=ident_f[:, :]), (b_ident,), (b_ident,))
        eps_c = sb("eps_c", [128, 1])
        S.op("pool", lambda e: e.memset(eps_c[:, :], EPS), (), (b_ident,))
        one_c = sb("one_c", [128, 1])
        S.op("pool", lambda e: e.memset(one_c[:, :], 1.0), (), (b_ident,))

        c_bf = sb("c_bf", [128, NK], BF16)
        with ExitStack() as ph:
            psb = lambda name, shape, dt=F32: ph.enter_context(nc.sbuf_tensor(name, list(shape), dt))
            c_sb = psb("c_sb", [128, NK])
            b_c = B("c")
            dma("sp", c_sb[:, :], I("cT")[:, :], (), (b_c,))
            S.op("act", lambda e: e.activation(out=c_bf[:, :], in_=c_sb[:, :], func=AF.Silu), (b_c,), (b_c,))
            NB0 = 2 * D // 512
            wsl = [psb("w0_%d" % i, [128, NK, 512], BF16) for i in range(2)]
            b_w = [B("w0_%d" % i) for i in range(2)]
            bsl = [psb("b0_%d" % i, [1, 512]) for i in range(2)]
            b_b = [B("b0_%d" % i) for i in range(2)]
            msl = [psb("m0_%d" % i, [1, 512]) for i in range(2)]
            b_m = [B("m0_%d" % i) for i in range(2)]
            ps0 = [ph.enter_context(nc.psum_tensor("ps0_%d" % i, [128, 512], F32)) for i in range(2)]
            b_ps = [B("ps0_%d" % i) for i in range(2)]
            w_ada_v = I("w_ada").rearrange("(k p) n -> p k n", p=128)
            for blk in range(NB0):
                s_ = blk % 2
                cs = slice(blk * 512, (blk + 1) * 512)
                dma("pool", wsl[s_][:, :, :], w_ada_v[:, :, cs], (), (b_w[s_],))
                dma("sp", bsl[s_][:, :], I("b_ada")[0:1, cs], (), (b_b[s_],))
                for k in range(NK):
                    S.op("pe", (lambda e, s_=s_, k=k: e.matmul(ps0[s_][0:1, :], lhsT=c_bf[:, k:k + 1],
                                                                rhs=wsl[s_][:, k, :], start=(k == 0),
                                                                stop=(k == NK - 1))),
                         (b_c, b_w[s_]), (b_ps[s_],))
                S.op("dve", (lambda e, s_=s_: e.tensor_tensor(out=msl[s_][:, :], in0=ps0[s_][0:1, :],
                                                               in1=bsl[s_][:, :], op=ALU.add)),
                     (b_ps[s_], b_b[s_]), (b_m[s_],))
                dma("sp", modrow[0:1, cs], msl[s_][:, :], (b_m[s_],), (b_modrow,))

        S.barrier()
        finals = [b_modrow]
        RSQ = lambda dst, src_, n: None

        def rstd(dst, ssum, n, bufs):
            S.op("act", (lambda e: e.activation(out=dst, in_=ssum, func=AF.Sqrt, bias=eps_c[:, 0:1], scale=1.0 / n)),
                 tuple(bufs) + (b_ident,), tuple(bufs))
            S.op("dve", (lambda e: e.reciprocal(out=dst, in_=dst)), tuple(bufs), tuple(bufs))

        def load_modT(psb, name, col0, gname):
            shT = psb(name + "_shT", [128, NK])
            scT = psb(name + "_scT", [128, NK])
            gT = psb(name + "_gT", [128, NK])
            A = psb(name + "_A", [128, NK])
            bm = B(name + "_mod")
            S.op("sp", lambda e: e.dma_start(out=shT[:, :], in_=modrow[0:1, col0:col0 + D].rearrange("o (k p) -> p (o k)", p=128),
                                             allow_slow_non_contiguous=True), (b_modrow,), (bm,), dma=True)
            S.op("sp", lambda e: e.dma_start(out=scT[:, :], in_=modrow[0:1, col0 + D:col0 + 2 * D].rearrange("o (k p) -> p (o k)", p=128),
                                             allow_slow_non_contiguous=True), (b_modrow,), (bm,), dma=True)
            dma("sp", gT[:, :], I(gname)[:, :], (), (bm,))
            S.op("dve", lambda e: e.scalar_tensor_tensor(out=A[:, :], in0=scT[:, :], scalar=1.0, in1=gT[:, :],
                                                         op0=ALU.add, op1=ALU.mult), (bm,), (bm,))
            return A, shT, bm

        def make_gemm(ph, psb, name, nslots=2):
            wsl = [psb(name + "_w%d" % i, [128, NK, 512], BF16) for i in range(nslots)]
            b_w = [B(name + "_w%d" % i) for i in range(nslots)]
            NST = 4
            stg = [psb(name + "_st%d" % i, [128, 512]) for i in range(NST)]
            b_st = [B(name + "_st%d" % i) for i in range(NST)]
            NPS = 4
            pacc = [ph.enter_context(nc.psum_tensor(name + "_pa%d" % i, [128, 512], F32)) for i in range(NPS)]
            b_pa = [B(name + "_pa%d" % i) for i in range(NPS)]
            state = {"cnt": 0, "blk": 0}

            def run(hT, b_hT, ntile, w_view, blocks, store):
                for bi, (c0, wd) in enumerate(blocks):
                    s_ = state["blk"] % nslots
                    state["blk"] += 1
                    dma("pool", wsl[s_][:, :, 0:wd], w_view[:, :, c0:c0 + wd], (), (b_w[s_],))
                    for t in range(ntile):
                        cnt = state["cnt"]
                        p_ = cnt % NPS
                        q_ = cnt % NST
                        for k in range(NK):
                            S.op("pe", (lambda e, p_=p_, s_=s_, k=k, t=t, wd=wd: e.matmul(
                                pacc[p_][:, 0:wd], lhsT=hT[:, k, t * 128:(t + 1) * 128], rhs=wsl[s_][:, k, 0:wd],
                                start=(k == 0), stop=(k == NK - 1))), (b_hT[t], b_w[s_]), (b_pa[p_],))
                        if cnt % 2 == 0:
                            S.op("act", (lambda e, p_=p_, q_=q_, wd=wd: e.activation(out=stg[q_][:, 0:wd], in_=pacc[p_][:, 0:wd], func=AF.Copy)),
                                 (b_pa[p_],), (b_st[q_],))
                        else:
                            S.op("dve", (lambda e, p_=p_, q_=q_, wd=wd: e.tensor_copy(out=stg[q_][:, 0:wd], in_=pacc[p_][:, 0:wd])),
                                 (b_pa[p_],), (b_st[q_],))
                        store(t, bi, stg[q_], b_st[q_])
                        state["cnt"] += 1
                    for _ in range(2):
                        if bg_tasks:
                            bg_tasks.pop(0)()
            return run

        def aprep_transposes(psT, b_psT, src_bf, b_src, hT, b_hTt, t, A=None, Bc=None, b_mod=None):
            for c4 in range(NK // 8):
                p_ = c4 % 2
                for j in range(8):
                    c = c4 * 8 + j
                    S.op("pe", (lambda e, p_=p_, j=j, c=c: e.transpose(out=psT[p_][:, j * 128:(j + 1) * 128],
                                                                     in_=src_bf[:, c * 128:(c + 1) * 128], identity=ident_bf[:, :])),
                         (b_src, b_ident), (b_psT[p_],))
                if A is None:
                    eng = "act" if c4 % 2 == 0 else "dve"
                    dst = hT[:, c4 * 8:(c4 + 1) * 8, t * 128:(t + 1) * 128]
                    srcv = psT[p_][:, :].rearrange("p (j n) -> p j n", n=128)
                    if eng == "act":
                        S.op("act", (lambda e, dst=dst, srcv=srcv: e.activation(out=dst, in_=srcv, func=AF.Copy)), (b_psT[p_],), (b_hTt,))
                    else:
                        S.op("dve", (lambda e, dst=dst, srcv=srcv: e.tensor_copy(out=dst, in_=srcv)), (b_psT[p_],), (b_hTt,))
                else:
                    for j in range(8):
                        c = c4 * 8 + j
                        dst = hT[:, c, t * 128:(t + 1) * 128]
                        srcv = psT[p_][:, j * 128:(j + 1) * 128]
                        if j % 2 == 0:
                            S.op("act", (lambda e, dst=dst, srcv=srcv, c=c: e.activation(out=dst, in_=srcv, func=AF.Identity,
                                                                                       bias=Bc[:, c:c + 1], scale=A[:, c:c + 1])),
                                 (b_psT[p_], b_mod), (b_hTt,))
                        else:
                            S.op("dve", (lambda e, dst=dst, srcv=srcv, c=c: e.tensor_scalar(out=dst, in0=srcv, scalar1=A[:, c:c + 1],
                                                                                          scalar2=Bc[:, c:c + 1], op0=ALU.mult, op1=ALU.add)),
                                 (b_psT[p_], b_mod), (b_hTt,))

        b_proj = [B("proj%d" % g) for g in range(NG)]
        b_pp = B("pp")
        roles = [(pu, 0, AW), (pva, AW, AW), (pq, 2 * AW, BKW), (pk, 2 * AW + BKW, BKW),
                 (pvb, 2 * AW + 2 * BKW, BW), (pr, 2 * AW + 2 * BKW + BW, BW), (pg, 2 * AW + 2 * BKW + 2 * BW, 16)]
        blocks1 = []
        for (arr, c0, wd) in roles:
            for o in range(0, wd, 512):
                blocks1.append((c0 + o, min(512, wd - o), arr, o))
        if phases >= 1:
            with ExitStack() as ph:
                psb = lambda name, shape, dt=F32: ph.enter_context(nc.sbuf_tensor(name, list(shape), dt))
                A1, B1, b_mod1 = load_modT(psb, "m1", 0, "gT_pre_mix")
                hT = psb("hT1", [128, NK, GT], BF16)
                xs = [psb("xs%d" % i, [128, D]) for i in range(2)]
                b_xs = [B("xs%d" % i) for i in range(2)]
                xn = [psb("xn%d" % i, [128, D], BF16) for i in range(2)]
                b_xn = [B("xn%d" % i) for i in range(2)]
                st1 = psb("st1", [128, 4])
                b_s1 = [B("st1_%d" % i) for i in range(2)]
                psT = [ph.enter_context(nc.psum_tensor("psT1_%d" % i, [128, 1024], BF16)) for i in range(2)]
                b_psT = [B("psT1_%d" % i) for i in range(2)]
                w_in_v = I("w_in").rearrange("(k p) n -> p k n", p=128)
                gemm1 = make_gemm(ph, psb, "g1")
                tw = [psb("tw%d" % i, [128, NK, 128], BF16) for i in range(2)]; b_tw = [B("tw%d" % i) for i in range(2)]
                tb = [psb("tb%d" % i, [1, 128]) for i in range(2)]; b_tb = [B("tb%d" % i) for i in range(2)]
                tm = [psb("tm%d" % i, [1, 128]) for i in range(2)]; b_tm = [B("tm%d" % i) for i in range(2)]
                pst = ph.enter_context(nc.psum_tensor("pst", [128, 128], F32)); b_pst = B("pst")
                w_ada_v1 = I("w_ada").rearrange("(k p) n -> p k n", p=128)
                NPC = 4 * D // 128

                def ada_issue(i):
                    s_ = i % 2
                    cs = slice(2 * D + i * 128, 2 * D + (i + 1) * 128)
                    dma("pool", tw[s_][:, :, :], w_ada_v1[:, :, cs], (), (b_tw[s_],))
                    dma("sp", tb[s_][:, :], I("b_ada")[0:1, cs], (), (b_tb[s_],))

                def ada_compute(i):
                    s_ = i % 2
                    cs = slice(2 * D + i * 128, 2 * D + (i + 1) * 128)
                    for k in range(NK):
                        S.op("pe", (lambda e, s_=s_, k=k: e.matmul(pst[0:1, :], lhsT=c_bf[:, k:k + 1], rhs=tw[s_][:, k, :],
                                                                    start=(k == 0), stop=(k == NK - 1))), (b_c, b_tw[s_]), (b_pst,))
                    S.op("dve", (lambda e, s_=s_: e.tensor_tensor(out=tm[s_][:, :], in0=pst[0:1, :], in1=tb[s_][:, :], op=ALU.add)),
                         (b_pst, b_tb[s_]), (b_tm[s_],))
                    dma("sp", modrow[0:1, cs], tm[s_][:, :], (b_tm[s_],), (b_modrow,))
                    if i + 2 < NPC:
                        ada_issue(i + 2)
                ada_issue(0)
                ada_issue(1)
                for i in range(NPC):
                    bg_tasks.append(lambda i=i: ada_compute(i))
                for g in range(NG):
                    b_hT = [B("hT1_%d_%d" % (g, t)) for t in range(G)]
                    for t in range(G):
                        s_ = t % 2
                        r0 = (g * G + t) * 128
                        dma("sp", xs[s_][:, :], I("x")[r0:r0 + 128, :], (), (b_xs[s_],))
                        ssv = st1[:, 2 * s_:2 * s_ + 1]
                        rsv = st1[:, 2 * s_ + 1:2 * s_ + 2]
                        S.op("dve", (lambda e, ssv=ssv: e.memset(ssv, 0.0)), (), (b_s1[s_],))
                        S.op("act", (lambda e, s_=s_, ssv=ssv: e.activation(out=xn[s_][:, :], in_=xs[s_][:, :], func=AF.Square, accum_out=ssv)),
                             (b_xs[s_],), (b_xn[s_], b_s1[s_]))
                        rstd(rsv, ssv, D, (b_s1[s_],))
                        S.op("act", (lambda e, s_=s_, rsv=rsv: e.activation(out=xn[s_][:, :], in_=xs[s_][:, :], func=AF.Copy, scale=rsv)),
                             (b_xs[s_], b_s1[s_]), (b_xn[s_],))
                        aprep_transposes(psT, b_psT, xn[s_], b_xn[s_], hT, b_hT[t], t, A1, B1, b_mod1)

                    def store1(t, bi, stage, b_stage, g=g):
                        c0, wd, arr, o = blocks1[bi]
                        r0 = (g * G + t) * 128
                        dma("sp", arr[r0:r0 + 128, o:o + wd], stage[:, 0:wd], (b_stage,), (b_proj[g],))
                    gemm1(hT, b_hT, G, w_in_v, [(b[0], b[1]) for b in blocks1], store1)
                blocksP = [b for b in blocks1 if b[2] in (pk, pvb, pg)]
                parr = {id(pk): ppk, id(pvb): ppv, id(pg): ppg}
                for gp in range(NP // G):
                    b_hT = [B("hTp_%d_%d" % (gp, t)) for t in range(G)]
                    for t in range(G):
                        s_ = t % 2
                        r0 = (gp * G + t) * 128
                        dma("sp", xs[s_][:, :], I("xprev")[r0:r0 + 128, :], (), (b_xs[s_],))
                        ssv = st1[:, 2 * s_:2 * s_ + 1]
                        rsv = st1[:, 2 * s_ + 1:2 * s_ + 2]
                        S.op("dve", (lambda e, ssv=ssv: e.memset(ssv, 0.0)), (), (b_s1[s_],))
                        S.op("act", (lambda e, s_=s_, ssv=ssv: e.activation(out=xn[s_][:, :], in_=xs[s_][:, :], func=AF.Square, accum_out=ssv)),
                             (b_xs[s_],), (b_xn[s_], b_s1[s_]))
                        rstd(rsv, ssv, D, (b_s1[s_],))
                        S.op("act", (lambda e, s_=s_, rsv=rsv: e.activation(out=xn[s_][:, :], in_=xs[s_][:, :], func=AF.Copy, scale=rsv)),
                             (b_xs[s_], b_s1[s_]), (b_xn[s_],))
                        aprep_transposes(psT, b_psT, xn[s_], b_xn[s_], hT, b_hT[t], t, A1, B1, b_mod1)

                    def storeP(t, bi, stage, b_stage, gp=gp):
                        c0, wd, arr, o = blocksP[bi]
                        r0 = (gp * G + t) * 128
                        dma("sp", parr[id(arr)][r0:r0 + 128, o:o + wd], stage[:, 0:wd], (b_stage,), (b_pp,))
                    gemm1(hT, b_hT, G, w_in_v, [(b[0], b[1]) for b in blocksP], storeP)
                while bg_tasks:
                    bg_tasks.pop(0)()
            finals += b_proj + [b_pp]

        b_ycat = [B("ycat%d" % g) for g in range(NG)]
        if phases >= 2:
            S.barrier()
            with ExitStack() as ph:
                psb = lambda name, shape, dt=F32: ph.enter_context(nc.sbuf_tensor(name, list(shape), dt))
                pps = lambda name, shape, dt=F32: ph.enter_context(nc.psum_tensor(name, list(shape), dt))
                b_c2 = B("c2")
                lng = psb("lng", [128, AW]); lnb = psb("lnb", [128, AW]); aog = psb("aog", [128, AW]); hgb = psb("hgb", [128, 512])
                dma("sp", lng[:, :], I("a_ln_g")[0:1, :].to_broadcast([128, AW]), (), (b_c2,))
                dma("sp", lnb[:, :], I("a_ln_b")[0:1, :].to_broadcast([128, AW]), (), (b_c2,))
                dma("sp", aog[:, :], I("a_out_g")[0:1, :].to_broadcast([128, AW]), (), (b_c2,))
                dma("sp", hgb[:, :], I("b_head_g")[0:1, :].to_broadcast([128, 512]), (), (b_c2,))
                bsT = psb("bsT", [128, 8])
                dma("sp", bsT[:, :], I("a_b_sT")[:, :], (), (b_c2,))
                wg2 = psb("wg2", [32, BKW])
                dma("sp", wg2[0:17, :], I("b_w_g2a")[:, :], (), (b_c2,))
                wsf = psb("wsf", [128, 8, 128])
                dma("sp", wsf[:, :, :], I("a_w_s").rearrange("h i j -> i h j"), (), (b_c2,))
                wsb = psb("wsb", [128, 8, 128], BF16)
                WT = psb("WT", [128, 8, 128], BF16)
                Um = psb("Um", [128, 128]); Lm = psb("Lm", [128, 128]); Mk = psb("Mk", [128, 128])
                negc = psb("negc", [128, 1])
                glT = psb("glT", [32, 128])
                Sf = psb("Sf", [128, 8, 512]); Sb = psb("Sb", [128, 8, 512], BF16)
                b_S = B("S")
                ISC = -1.0 / 16.0
                for h in range(8):
                    S.op("pool", (lambda e, h=h: e.affine_select(out=wsf[:, h, :], in_=wsf[:, h, :], pattern=[[-1, 128]],
                                                                 compare_op=ALU.is_ge, fill=0.0, base=0, channel_multiplier=1)),
                         (b_c2,), (b_c2,))
                S.op("pool", lambda e: e.tensor_copy(out=wsb[:, :, :], in_=wsf[:, :, :]), (b_c2,), (b_c2,))
                S.op("pool", lambda e: e.memset(Um[:, :], ISC), (), (b_c2,))
                S.op("pool", lambda e: e.affine_select(out=Um[:, :], in_=Um[:, :], pattern=[[1, 128]], compare_op=ALU.is_ge,
                                                       fill=0.0, base=0, channel_multiplier=-1), (b_c2,), (b_c2,))
                S.op("pool", lambda e: e.memset(Lm[:, :], ISC), (), (b_c2,))
                S.op("pool", lambda e: e.affine_select(out=Lm[:, :], in_=Lm[:, :], pattern=[[-1, 128]], compare_op=ALU.is_gt,
                                                       fill=0.0, base=0, channel_multiplier=1), (b_c2,), (b_c2,))
                S.op("pool", lambda e: e.memset(Mk[:, :], 1.0), (), (b_c2,))
                S.op("pool", lambda e: e.affine_select(out=Mk[:, :], in_=Mk[:, :], pattern=[[1, 128]], compare_op=ALU.is_ge,
                                                       fill=0.0, base=0, channel_multiplier=-1), (b_c2,), (b_c2,))
                S.op("pool", lambda e: e.memset(negc[:, :], ISC), (), (b_c2,))
                S.op("pool", lambda e: e.memset(glT[:, :], 1.0), (), (b_c2,))
                S.op("pool", lambda e: e.memset(Sf[:, :, :], 0.0), (), (b_S,))
                S.op("pool", lambda e: e.memset(Sb[:, :, :], 0.0), (), (b_S,))
                pA = [pps("pA%d" % i, [128, 512]) for i in range(2)]; b_pA = [B("pA%d" % i) for i in range(2)]
                P1 = pps("P1", [128, 1024]); b_P1 = B("P1")
                P2 = pps("P2", [128, 1024]); b_P2 = B("P2")
                pT = pps("pT2", [128, 1024], BF16); b_pT = B("pT2")
                pX = pps("pX", [128, 512]); b_pX = B("pX")
                for h in range(8):
                    S.op("pe", (lambda e, h=h: e.transpose(out=pT[:, h * 128:(h + 1) * 128], in_=wsb[:, h, :], identity=ident_bf[:, :])),
                         (b_c2, b_ident), (b_pT,))
                S.op("dve", lambda e: e.tensor_copy(out=WT[:, :, :], in_=pT[:, :].rearrange("p (h n) -> p h n", n=128)), (b_pT,), (b_c2,))
                u = psb("u", [128, AW]); va = psb("va", [128, AW]); vb = psb("vb", [128, BW]); r_ = psb("r", [128, BW])
                b_u = B("u"); b_va = B("va"); b_vb = B("vb"); b_r = B("r")
                q = [psb("q%d" % i, [128, BKW]) for i in range(2)]; kk = [psb("k%d" % i, [128, BKW]) for i in range(2)]
                gg = [psb("g%d" % i, [128, 16]) for i in range(2)]
                b_q = [B("q%d" % i) for i in range(2)]; b_k = [B("k%d" % i) for i in range(2)]; b_g = [B("g%d" % i) for i in range(2)]
                tA = psb("tA", [128, AW]); b_tA = B("tA")
                vn = psb("vn", [128, AW], BF16); b_vn = B("vn")
                yA = psb("yA", [128, AW]); b_yA = B("yA")
                yc = [psb("yc%d" % i, [128, D], BF16) for i in range(2)]; b_yc = [B("yc%d" % i) for i in range(2)]
                st2 = psb("st2", [128, 16]); b_st2 = B("st2"); b_st2b = B("st2b"); b_st2c = B("st2c")
                esb = psb("esb", [128, BKW]); b_esb = B("esb")
                la = psb("la", [128, BKW]); b_la = B("la")
                E = [psb("E%d" % i, [128, BKW]) for i in range(2)]; b_E = [B("E%d" % i) for i in range(2)]
                qd = psb("qd", [128, BKW], BF16); kd = psb("kd", [128, BKW], BF16); ks = psb("ks", [128, BKW], BF16)
                b_qd = B("qd"); b_kd = B("kd"); b_ks = B("ks")
                qdT = psb("qdT", [128, 8, 128], BF16); kdT = psb("kdT", [128, 8, 128], BF16); b_qdT = B("qdT"); b_kdT = B("kdT")
                vbf = psb("vbf", [128, BW], BF16); b_vbf = B("vbf")
                sr = psb("sr", [128, BW]); b_sr = B("sr")
                dec = psb("dec", [128, 8]); b_dec = B("dec")
                att = [psb("att%d" % i, [128, 128], BF16) for i in range(2)]; b_att = [B("att%d" % i) for i in range(2)]
                tn = [psb("tn%d" % i, [128, 512]) for i in range(2)]; b_tn = [B("tn%d" % i) for i in range(2)]
                so = psb("so", [128, 8]); b_so = [B("so%d" % i) for i in range(4)]

                pmask = psb("pmask_sb", [128, NP])
                dma("sp", pmask[:, :], I("pmask")[:, :], (), (b_c2,))
                items = [("p", j) for j in range(NP)] + [("m", j) for j in range(nt)]

                def ld_A(ii):
                    kind, tj = items[ii]
                    if kind != "m":
                        return
                    rw = slice(tj * 128, tj * 128 + 128); gj = tj // G
                    dma("sp", u[:, :], pu[rw, :], (b_proj[gj],), (b_u,))
                    dma("sp", va[:, :], pva[rw, :], (b_proj[gj],), (b_va,))

                def ld_qkg(ii):
                    kind, tj = items[ii]
                    rw = slice(tj * 128, tj * 128 + 128); gj = tj // G; sj = ii % 2
                    if kind == "m":
                        dma("sp", gg[sj][:, :], pg[rw, :], (b_proj[gj],), (b_g[sj],))
                        dma("sp", q[sj][:, :], pq[rw, :], (b_proj[gj],), (b_q[sj],))
                        dma("sp", kk[sj][:, :], pk[rw, :], (b_proj[gj],), (b_k[sj],))
                    else:
                        dma("sp", gg[sj][:, :], ppg[rw, :], (b_pp,), (b_g[sj],))
                        dma("sp", kk[sj][:, :], ppk[rw, :], (b_pp,), (b_k[sj],))

                def ld_B(ii):
                    kind, tj = items[ii]
                    rw = slice(tj * 128, tj * 128 + 128); gj = tj // G
                    if kind == "m":
                        dma("sp", vb[:, :], pvb[rw, :], (b_proj[gj],), (b_vb,))
                        dma("sp", r_[:, :], pr[rw, :], (b_proj[gj],), (b_r,))
                    else:
                        dma("sp", vb[:, :], ppv[rw, :], (b_pp,), (b_vb,))

                nit = len(items)
                for ii, (kind, ti) in enumerate(items):
                    so_ = (kind == "p")
                    g = ti // G
                    r0 = ti * 128
                    s_ = ii % 2
                    rows = slice(r0, r0 + 128)
                    if ii == 0:
                        ld_A(0); ld_qkg(0); ld_B(0)
                    if ii + 1 < nit:
                        ld_qkg(ii + 1)
                    if so_ and ii + 1 < nit and items[ii + 1][0] == "m":
                        ld_A(ii + 1)
                    S.mute = so_
                    s1 = st2[:, 0:1]; s2 = st2[:, 1:2]; mean = st2[:, 2:3]; msq = st2[:, 3:4]; var = st2[:, 4:5]
                    rs = st2[:, 5:6]; nmr = st2[:, 6:7]; ssy = st2[:, 7:8]; rsy = st2[:, 8:9]
                    S.op("dve", lambda e: e.memset(st2[:, 0:2], 0.0), (), (b_st2,))
                    S.op("act", lambda e: e.activation(out=tA[:, :], in_=va[:, :], func=AF.Identity, accum_out=st2[:, 0:1]),
                         (b_va, b_c2), (b_tA, b_st2))
                    S.op("act", lambda e: e.activation(out=tA[:, :], in_=va[:, :], func=AF.Square, accum_out=st2[:, 1:2]),
                         (b_va,), (b_tA, b_st2))

                    S.op("dve", lambda e: e.tensor_scalar(out=st2[:, 2:3], in0=st2[:, 0:1], scalar1=1.0 / AW, scalar2=None, op0=ALU.mult),
                         (b_st2,), (b_st2,))
                    S.op("dve", lambda e: e.tensor_tensor(out=st2[:, 3:4], in0=st2[:, 2:3], in1=st2[:, 2:3], op=ALU.mult), (b_st2,), (b_st2,))
                    S.op("dve", lambda e: e.scalar_tensor_tensor(out=st2[:, 4:5], in0=st2[:, 1:2], scalar=1.0 / AW, in1=st2[:, 3:4],
                                                                 op0=ALU.mult, op1=ALU.subtract), (b_st2,), (b_st2,))
                    rstd(st2[:, 5:6], st2[:, 4:5], 1.0, (b_st2,))
                    S.op("dve", lambda e: e.scalar_tensor_tensor(out=st2[:, 6:7], in0=st2[:, 2:3], scalar=-1.0, in1=st2[:, 5:6],
                                                                 op0=ALU.mult, op1=ALU.mult), (b_st2,), (b_st2,))
                    S.op("act", lambda e: e.activation(out=tA[:, :], in_=va[:, :], func=AF.Identity, bias=st2[:, 6:7], scale=st2[:, 5:6]),
                         (b_va, b_st2), (b_tA,))
                    S.op("dve", lambda e: e.tensor_tensor(out=tA[:, :], in0=tA[:, :], in1=lng[:, :], op=ALU.mult), (b_tA, b_c2), (b_tA,))
                    S.op("dve", lambda e: e.tensor_tensor(out=vn[:, :], in0=tA[:, :], in1=lnb[:, :], op=ALU.add), (b_tA, b_c2), (b_vn,))
                    for h in range(8):
                        p_ = (h // 2) % 2
                        o_ = (h % 2) * 256
                        S.op("pe", (lambda e, h=h, p_=p_, o_=o_: e.matmul(pA[p_][:, o_:o_ + 256], lhsT=WT[:, h, :], rhs=vn[:, h * 256:(h + 1) * 256],
                                                                        start=True, stop=True)), (b_vn, b_c2), (b_pA[p_],))
                        S.op("dve", (lambda e, h=h, p_=p_, o_=o_: e.scalar_tensor_tensor(out=yA[:, h * 256:(h + 1) * 256], in0=pA[p_][:, o_:o_ + 256],
                                                                                       scalar=bsT[:, h:h + 1], in1=u[:, h * 256:(h + 1) * 256],
                                                                                       op0=ALU.add, op1=ALU.mult)),
                             (b_pA[p_], b_u, b_c2), (b_yA,))
                    if ii + 1 < nit:
                        ld_A(ii + 1)
                    S.op("dve", lambda e: e.memset(st2[:, 7:8], 0.0), (), (b_st2b,))
                    S.op("act", lambda e: e.activation(out=tA[:, :], in_=yA[:, :], func=AF.Square, accum_out=st2[:, 7:8]),
                         (b_yA,), (b_tA, b_st2b))
                    rstd(st2[:, 8:9], st2[:, 7:8], AW, (b_st2b,))
                    S.op("dve", (lambda e, s_=s_: e.scalar_tensor_tensor(out=yc[s_][:, 0:AW], in0=yA[:, :], scalar=st2[:, 8:9], in1=aog[:, :],
                                                                        op0=ALU.mult, op1=ALU.mult)), (b_yA, b_st2b, b_c2), (b_yc[s_],), hard=True)
                    S.mute = False
                    S.op("pe", (lambda e, s_=s_: e.transpose(out=pX[0:16, 0:128], in_=gg[s_][:, :], identity=ident_f[:, :])),
                         (b_g[s_], b_ident), (b_pX,))
                    S.op("dve", lambda e: e.tensor_copy(out=glT[0:16, :], in_=pX[0:16, 0:128]), (b_pX, b_c2), (b_c2,))
                    for hf in range(2):
                        S.op("pe", (lambda e, hf=hf: e.matmul(P1[:, hf * 512:(hf + 1) * 512], lhsT=glT[0:17, :], rhs=wg2[0:17, hf * 512:(hf + 1) * 512],
                                                              start=True, stop=True)), (b_c2,), (b_P1,))
                    S.op("act", lambda e: e.activation(out=esb[:, :], in_=P1[:, :], func=AF.Exp, scale=-1.0), (b_P1,), (b_esb,))
                    S.op("act", lambda e: e.activation(out=la[:, :], in_=esb[:, :], func=AF.Ln, bias=one_c[:, 0:1], scale=1.0), (b_esb, b_ident), (b_la,))
                    S.mute = so_
                    for hf in range(2):
                        S.op("pe", (lambda e, hf=hf: e.matmul(P2[:, hf * 512:(hf + 1) * 512], lhsT=Um[:, :], rhs=la[:, hf * 512:(hf + 1) * 512],
                                                              start=True, stop=True)), (b_la, b_c2), (b_P2,))
                    S.mute = False
                    for hf in range(2):
                        S.op("pe", (lambda e, hf=hf: e.matmul(P1[:, hf * 512:(hf + 1) * 512], lhsT=Lm[:, :], rhs=la[:, hf * 512:(hf + 1) * 512],
                                                              start=True, stop=True)), (b_la, b_c2), (b_P1,))
                    for c in range(8):
                        S.op("pe", (lambda e, c=c: e.matmul(pX[:, 256 + c:257 + c], lhsT=la[:, c * 128:(c + 1) * 128], rhs=negc[:, 0:1],
                                                            start=True, stop=True)), (b_la, b_c2), (b_pX,))
                    S.op("act", lambda e: e.activation(out=dec[:, :], in_=pX[:, 256:264], func=AF.Exp), (b_pX,), (b_dec,))
                    S.mute = so_
                    S.op("act", lambda e: e.activation(out=E[0][:, :], in_=P2[:, :], func=AF.Exp), (b_P2,), (b_E[0],))
                    S.op("dve", (lambda e, s_=s_: e.scalar_tensor_tensor(out=qd[:, :], in0=q[s_][:, :], scalar=0.0625, in1=E[0][:, :],
                                                                        op0=ALU.mult, op1=ALU.mult)), (b_q[s_], b_E[0]), (b_qd,))
                    S.op("act", lambda e: e.activation(out=E[1][:, :], in_=P2[:, :], func=AF.Exp, scale=-1.0), (b_P2,), (b_E[1],))
                    S.op("dve", (lambda e, s_=s_: e.tensor_tensor(out=kd[:, :], in0=kk[s_][:, :], in1=E[1][:, :], op=ALU.mult)),
                         (b_k[s_], b_E[1]), (b_kd,))
                    S.mute = False
                    S.op("act", lambda e: e.activation(out=E[0][:, :], in_=P1[:, :], func=AF.Exp), (b_P1,), (b_E[0],))
                    S.op("dve", (lambda e, s_=s_: e.tensor_tensor(out=ks[:, :], in0=kk[s_][:, :], in1=E[0][:, :], op=ALU.mult)),
                         (b_k[s_], b_E[0]), (b_ks,))
                    if so_:
                        S.op("act", (lambda e, ti=ti: e.activation(out=vbf[:, :], in_=vb[:, :], func=AF.Copy, scale=pmask[:, ti:ti + 1])),
                             (b_vb, b_c2), (b_vbf,))
                    else:
                        S.op("act", lambda e: e.activation(out=vbf[:, :], in_=vb[:, :], func=AF.Copy), (b_vb,), (b_vbf,))
                    S.mute = so_
                    S.op("act", lambda e: e.activation(out=sr[:, :], in_=r_[:, :], func=AF.Silu), (b_r,), (b_sr,))
                    S.mute = False
                    if ii + 1 < nit:
                        ld_B(ii + 1)
                    S.mute = so_
                    for (srcb, b_src, dstT, b_dst, eng) in ((qd, b_qd, qdT, b_qdT, "act"), (kd, b_kd, kdT, b_kdT, "dve")):
                        for c in range(8):
                            S.op("pe", (lambda e, c=c, srcb=srcb: e.transpose(out=pT[:, c * 128:(c + 1) * 128], in_=srcb[:, c * 128:(c + 1) * 128],
                                                                            identity=ident_bf[:, :])), (b_src, b_ident), (b_pT,))
                        if eng == "act":
                            S.op("act", (lambda e, dstT=dstT: e.activation(out=dstT[:, :, :], in_=pT[:, :].rearrange("p (h n) -> p h n", n=128), func=AF.Copy)),
                                 (b_pT,), (b_dst,))
                        else:
                            S.op("dve", (lambda e, dstT=dstT: e.tensor_copy(out=dstT[:, :, :], in_=pT[:, :].rearrange("p (h n) -> p h n", n=128))),
                                 (b_pT,), (b_dst,))
                    for h in range(4):
                        a_ = h % 2
                        S.mute = so_
                        for c in range(2):
                            S.op("pe", (lambda e, h=h, c=c: e.matmul(pX[:, 0:128], lhsT=kdT[:, 2 * h + c, :], rhs=qdT[:, 2 * h + c, :],
                                                                   start=(c == 0), stop=(c == 1))), (b_kdT, b_qdT), (b_pX,))
                        S.op("dve", (lambda e, a_=a_: e.tensor_tensor(out=att[a_][:, :], in0=pX[:, 0:128], in1=Mk[:, :], op=ALU.mult)),
                             (b_pX, b_c2), (b_att[a_],))
                        S.op("pe", (lambda e, h=h, a_=a_: e.matmul(pA[a_][:, :], lhsT=att[a_][:, :], rhs=vbf[:, h * 512:(h + 1) * 512],
                                                                 start=True, stop=False)), (b_att[a_], b_vbf), (b_pA[a_],))
                        for c in range(2):
                            S.op("pe", (lambda e, h=h, c=c, a_=a_: e.matmul(pA[a_][:, :], lhsT=qdT[:, 2 * h + c, :], rhs=Sb[:, 2 * h + c, :],
                                                                          start=False, stop=(c == 1))), (b_qdT, b_S), (b_pA[a_],))
                        S.mute = False
                        for c in range(2):
                            ch = 2 * h + c
                            S.op("pe", (lambda e, h=h, ch=ch, c=c: e.matmul(P2[:, c * 512:(c + 1) * 512], lhsT=ks[:, ch * 128:(ch + 1) * 128],
                                                                          rhs=vbf[:, h * 512:(h + 1) * 512], start=True, stop=True)),
                                 (b_ks, b_vbf), (b_P2,))
                            S.op("dve", (lambda e, ch=ch, c=c: e.scalar_tensor_tensor(out=Sf[:, ch, :], in0=Sf[:, ch, :], scalar=dec[:, ch:ch + 1],
                                                                                    in1=P2[:, c * 512:(c + 1) * 512], op0=ALU.mult, op1=ALU.add)),
                                 (b_P2, b_dec, b_S), (b_S,))
                            if (not so_) or ii == NP - 1:
                                S.op("act", (lambda e, ch=ch: e.activation(out=Sb[:, ch, :], in_=Sf[:, ch, :], func=AF.Copy)), (b_S,), (b_S,))
                        S.mute = so_
                        S.op("dve", (lambda e, h=h: e.memset(so[:, 2 * h:2 * h + 1], 0.0)), (), (b_so[h],))
                        S.op("act", (lambda e, h=h, a_=a_: e.activation(out=tn[a_][:, :], in_=pA[a_][:, :], func=AF.Square, accum_out=so[:, 2 * h:2 * h + 1])),
                             (b_pA[a_],), (b_tn[a_], b_so[h]))
                        rstd(so[:, 2 * h + 1:2 * h + 2], so[:, 2 * h:2 * h + 1], 512, (b_so[h],))
                        S.op("dve", (lambda e, h=h, a_=a_: e.scalar_tensor_tensor(out=tn[a_][:, :], in0=pA[a_][:, :], scalar=so[:, 2 * h + 1:2 * h + 2],
                                                                                in1=hgb[:, :], op0=ALU.mult, op1=ALU.mult)),
                             (b_pA[a_], b_so[h], b_c2), (b_tn[a_],), hard=True)
                        S.op("dve", (lambda e, h=h, a_=a_, s_=s_: e.tensor_tensor(out=yc[s_][:, AW + h * 512:AW + (h + 1) * 512], in0=tn[a_][:, :],
                                                                                 in1=sr[:, h * 512:(h + 1) * 512], op=ALU.mult)),
                             (b_tn[a_], b_sr), (b_yc[s_],))
                    if not so_:
                        dma("sp", ycat[rows, :], yc[s_][:, :], (b_yc[s_],), (b_ycat[g],))
                    S.mute = False
                    if ti == 0 and not so_:
                        dbg("st2", st2[:, :], b_st2b, [128, 16]); dbg("vn", vn[:, :], b_vn, [128, AW], BF16)
                        dbg("yA", yA[:, :], b_yA, [128, AW]); dbg("la", la[:, :], b_la, [128, BKW])
                        dbg("qd", qd[:, :], b_qd, [128, BKW], BF16); dbg("kd", kd[:, :], b_kd, [128, BKW], BF16)
                        dbg("ks", ks[:, :], b_ks, [128, BKW], BF16); dbg("dec", dec[:, :], b_dec, [128, 8])
                        dbg("qdT", qdT[:, :, :], b_qdT, [128, 8, 128], BF16); dbg("att1", att[1][:, :], b_att[1], [128, 128], BF16)
                        dbg("tn1", tn[1][:, :], b_tn[1], [128, 512]); dbg("so", so[:, :], b_so[3], [128, 8])
                        dbg("WT", WT[:, :, :], b_c2, [128, 8, 128], BF16); dbg("Um", Um[:, :], b_c2, [128, 128])
                        dbg("Lm", Lm[:, :], b_c2, [128, 128]); dbg("Mk", Mk[:, :], b_c2, [128, 128])
                        dbg("glT", glT[:, :], b_c2, [32, 128]); dbg("Sf", Sf[:, :, :], b_S, [128, 8, 512])
                        dbg("sr", sr[:, :], b_sr, [128, BW]); dbg("vbf", vbf[:, :], b_vbf, [128, BW], BF16)
                        dbg("lng", lng[:, :], b_c2, [128, AW])
            finals += b_ycat

        b_yraw = [B("yraw%d" % g) for g in range(NG)]
        if phases >= 3:
            S.barrier()
            with ExitStack() as ph:
                psb = lambda name, shape, dt=F32: ph.enter_context(nc.sbuf_tensor(name, list(shape), dt))
                yT = psb("yT3", [128, NK, GT], BF16)
                yl = [psb("yl%d" % i, [128, D], BF16) for i in range(2)]
                b_yl = [B("yl%d" % i) for i in range(2)]
                psT = [ph.enter_context(nc.psum_tensor("psT3_%d" % i, [128, 1024], BF16)) for i in range(2)]
                b_psT = [B("psT3_%d" % i) for i in range(2)]
                w_out_v = I("w_out").rearrange("(k p) n -> p k n", p=128)
                gemm3 = make_gemm(ph, psb, "g3")
                for g in range(NG):
                    b_yT = [B("yT3_%d_%d" % (g, t)) for t in range(G)]
                    for t in range(G):
                        s_ = t % 2
                        r0 = (g * G + t) * 128
                        dma("sp", yl[s_][:, :], ycat[r0:r0 + 128, :], (b_ycat[g],), (b_yl[s_],))
                        aprep_transposes(psT, b_psT, yl[s_], b_yl[s_], yT, b_yT[t], t)

                    def store3(t, bi, stage, b_stage, g=g):
                        r0 = (g * G + t) * 128
                        dma("sp", yraw[r0:r0 + 128, bi * 512:(bi + 1) * 512], stage[:, :], (b_stage,), (b_yraw[g],))
                    gemm3(yT, b_yT, G, w_out_v, [(n * 512, 512) for n in range(D // 512)], store3)
            finals += b_yraw

        def load_gate_bc(psb, dst, tmp, col0, gname, bbuf, add_one=False):
            dma("sp", dst[:, :], modrow[0:1, col0:col0 + D].to_broadcast([128, D]), (b_modrow,), (bbuf,))
            if add_one:
                S.op("dve", lambda e: e.tensor_scalar(out=dst[:, :], in0=dst[:, :], scalar1=1.0, scalar2=None, op0=ALU.add), (bbuf,), (bbuf,))
            bt = B("gbc_tmp")
            dma("sp", tmp[:, :], I(gname)[0:1, :].to_broadcast([128, D]), (), (bt,))
            S.op("pool", lambda e: e.tensor_tensor(out=dst[:, :], in0=dst[:, :], in1=tmp[:, :], op=ALU.mult), (bbuf, bt), (bbuf,))
            return bt

        NSLOT = NE * CAP
        slot_tok = dscr("slot_tok", [NSLOT, 1], I32)
        b_x1 = B("x1"); b_xn2 = B("xn2"); b_slot_tok = B("slot_tok")
        rt_keep = None
        if phases >= 4:
            S.barrier()
            ph4 = top
            slot_i = sb("slot_i", [128, nt, 8], I32)
            w_all = sb("w_all", [128, nt, 8])
            b_tab = B("tab")
            with ExitStack() as ph:
                psb = lambda name, shape, dt=F32: ph.enter_context(nc.sbuf_tensor(name, list(shape), dt))
                pps = lambda name, shape, dt=F32: ph.enter_context(nc.psum_tensor(name, list(shape), dt))
                A2, B2, b_mod2 = load_modT(psb, "m2", 3 * D, "gT_pre_ffn")
                G1 = psb("G1", [128, D]); b_G1 = B("G1")
                yr = [psb("yr%d" % i, [128, D]) for i in range(2)]; b_yr = [B("yr%d" % i) for i in range(2)]
                xx = [psb("xx%d" % i, [128, D]) for i in range(2)]; b_xx = [B("xx%d" % i) for i in range(2)]
                xnb = psb("xnb", [128, D], BF16); b_xnb = B("xnb")
                h2Tf = psb("h2Tf", [128, NK, 128]); b_h2Tf = B("h2Tf")
                load_gate_bc(psb, G1, yr[1], 2 * D, "g_post_mix", b_G1)
                b_yr[1].W = list(b_G1.W); b_yr[1].R = list(b_G1.R)
                A2bc = psb("A2bc", [128, D]); B2bc = psb("B2bc", [128, D]); b_m2bc = B("m2bc")
                load_gate_bc(psb, A2bc, yr[0], 4 * D, "g_pre_ffn", b_m2bc, add_one=True)
                b_yr[0].W = list(b_m2bc.W); b_yr[0].R = list(b_m2bc.R)
                dma("sp", B2bc[:, :], modrow[0:1, 3 * D:4 * D].to_broadcast([128, D]), (b_modrow,), (b_m2bc,))
                wr = psb("wr", [128, NK, NE]); b_c4 = B("c4")
                dma("sp", wr[:, :, :], I("w_router").rearrange("(k p) e -> p k e", p=128), (), (b_c4,))
                rb = psb("rb", [128, NE])
                dma("sp", rb[:, :], I("router_bias")[0:1, :].to_broadcast([128, NE]), (), (b_c4,))
                SLT = psb("SLT", [128, 128], BF16); ONES = psb("ONES", [128, 128], BF16)
                tmpf = psb("tmpf", [128, 128])
                S.op("pool", lambda e: e.memset(tmpf[:, :], 1.0), (), (b_c4,))
                S.op("pool", lambda e: e.affine_select(out=tmpf[:, :], in_=tmpf[:, :], pattern=[[1, 128]], compare_op=ALU.is_gt,
                                                       fill=0.0, base=0, channel_multiplier=-1), (b_c4,), (b_c4,))
                S.op("pool", lambda e: e.tensor_copy(out=SLT[:, :], in_=tmpf[:, :]), (b_c4,), (b_c4,))
                S.op("pool", lambda e: e.memset(ONES[:, :], 1.0), (), (b_c4,))
                eoff = psb("eoff", [128, NE])
                S.op("pool", lambda e: e.iota(eoff[:, :], [[CAP, NE]], base=0, channel_multiplier=0,
                                              allow_small_or_imprecise_dtypes=True), (), (b_c4,))
                tokid = psb("tokid", [128, nt], I32)
                S.op("pool", lambda e: e.iota(tokid[:, :], [[128, nt]], base=0, channel_multiplier=1), (), (b_c4,))
                cntb = psb("cntb", [128, NE]); b_cnt = B("cntb")
                S.op("pool", lambda e: e.memset(cntb[:, :], 0.0), (), (b_cnt,))
                zt = psb("zt", [128, NSLOT // 128], I32)
                S.op("pool", lambda e: e.memset(zt[:, :], 0), (), (b_c4,))
                dma("sp", slot_tok.rearrange("(p n) o -> p (n o)", p=128), zt[:, :], (b_c4,), (b_slot_tok,))
                st4 = psb("st4", [128, 8]); b_st4 = [B("st4_%d" % i) for i in range(2)]
                sc = psb("sc", [128, NE]); sel = psb("sel", [128, NE]); tmp64 = psb("tmp64", [128, NE]); eq = psb("eq", [128, NE])
                msel = psb("msel", [128, NE]); Mm = psb("Mm", [128, NE]); Mb = psb("Mb", [128, NE], BF16); Wt = psb("Wt", [128, NE])
                pos = psb("pos", [128, NE]); okM = psb("okM", [128, NE]); slotf = psb("slotf", [128, NE])
                sm = psb("sm", [128, 64])
                scat_f = psb("scat_f", [128, 8]); scat_i = psb("scat_i", [128, 8], I32)
                slot_f = psb("slot_f", [128, 8])
                b_rt = B("rt"); b_scat = B("scat")
                psTf = [pps("psTf%d" % i, [128, 512]) for i in range(2)]; b_psTf = [B("psTf%d" % i) for i in range(2)]
                psR = pps("psR", [128, NE]); b_psR = B("psR")
                psP = pps("psP", [128, 128]); b_psP = B("psP")
                BIG = 1.0e9
                for ti in range(nt):
                    g = ti // G
                    s_ = ti % 2
                    rows = slice(ti * 128, ti * 128 + 128)
                    dma("sp", yr[s_][:, :], yraw[rows, :], (b_yraw[g],), (b_yr[s_],))
                    dma("sp", xx[s_][:, :], I("x")[rows, :], (), (b_xx[s_],))
                    ssv = st4[:, 4 * s_:4 * s_ + 1]; rsv = st4[:, 4 * s_ + 1:4 * s_ + 2]
                    ss2 = st4[:, 4 * s_ + 2:4 * s_ + 3]; rs2 = st4[:, 4 * s_ + 3:4 * s_ + 4]
                    S.op("dve", (lambda e, s_=s_: e.memset(st4[:, 4 * s_:4 * s_ + 4], 0.0)), (), (b_st4[s_],))
                    S.op("act", (lambda e, s_=s_, ssv=ssv: e.activation(out=xnb[:, :], in_=yr[s_][:, :], func=AF.Square, accum_out=ssv)),
                         (b_yr[s_],), (b_xnb, b_st4[s_]))
                    rstd(rsv, ssv, D, (b_st4[s_],))
                    S.op("dve", (lambda e, s_=s_, rsv=rsv: e.scalar_tensor_tensor(out=yr[s_][:, :], in0=yr[s_][:, :], scalar=rsv, in1=G1[:, :],
                                                                                 op0=ALU.mult, op1=ALU.mult)), (b_yr[s_], b_st4[s_], b_G1), (b_yr[s_],))
                    S.op("dve", (lambda e, s_=s_: e.tensor_tensor(out=xx[s_][:, :], in0=xx[s_][:, :], in1=yr[s_][:, :], op=ALU.add)),
                         (b_xx[s_], b_yr[s_]), (b_xx[s_],))
                    dma("sp", x1[rows, :], xx[s_][:, :], (b_xx[s_],), (b_x1,))
                    S.op("act", (lambda e, s_=s_, ss2=ss2: e.activation(out=xnb[:, :], in_=xx[s_][:, :], func=AF.Square, accum_out=ss2)),
                         (b_xx[s_],), (b_xnb, b_st4[s_]))
                    rstd(rs2, ss2, D, (b_st4[s_],))
                    S.op("act", (lambda e, s_=s_, rs2=rs2: e.activation(out=yr[s_][:, :], in_=xx[s_][:, :], func=AF.Copy, scale=rs2)),
                         (b_xx[s_], b_st4[s_]), (b_yr[s_],))
                    S.op("dve", (lambda e, s_=s_: e.tensor_tensor(out=xx[s_][:, :], in0=yr[s_][:, :], in1=A2bc[:, :], op=ALU.mult)),
                         (b_yr[s_], b_m2bc), (b_xx[s_],))
                    S.op("dve", (lambda e, s_=s_: e.tensor_tensor(out=xnb[:, :], in0=xx[s_][:, :], in1=B2bc[:, :], op=ALU.add)),
                         (b_xx[s_], b_m2bc), (b_xnb,))
                    dma("sp", xn2[rows, :], xnb[:, :], (b_xnb,), (b_xn2,))
                    for c4 in range(NK // 4):
                        p_ = c4 % 2
                        for j in range(4):
                            c = c4 * 4 + j
                            S.op("pe", (lambda e, p_=p_, j=j, c=c, s_=s_: e.transpose(out=psTf[p_][:, j * 128:(j + 1) * 128],
                                                                                    in_=yr[s_][:, c * 128:(c + 1) * 128], identity=ident_f[:, :])),
                                 (b_yr[s_], b_ident), (b_psTf[p_],))
                        for j in range(4):
                            c = c4 * 4 + j
                            if j % 2 == 0:
                                S.op("act", (lambda e, p_=p_, j=j, c=c: e.activation(out=h2Tf[:, c, :], in_=psTf[p_][:, j * 128:(j + 1) * 128], func=AF.Identity,
                                                                                   bias=B2[:, c:c + 1], scale=A2[:, c:c + 1])), (b_psTf[p_], b_mod2), (b_h2Tf,))
                            else:
                                S.op("dve", (lambda e, p_=p_, j=j, c=c: e.tensor_scalar(out=h2Tf[:, c, :], in0=psTf[p_][:, j * 128:(j + 1) * 128], scalar1=A2[:, c:c + 1],
                                                                                      scalar2=B2[:, c:c + 1], op0=ALU.mult, op1=ALU.add)), (b_psTf[p_], b_mod2), (b_h2Tf,))
                    for k in range(NK):
                        S.op("pe", (lambda e, k=k: e.matmul(psR[:, :], lhsT=h2Tf[:, k, :], rhs=wr[:, k, :], start=(k == 0), stop=(k == NK - 1))),
                             (b_h2Tf, b_c4), (b_psR,))
                    R_ = lambda fn, extra_r=(), extra_w=(): S.op("dve", fn, (b_rt, b_c4) + tuple(extra_r), (b_rt,) + tuple(extra_w))
                    S.op("act", lambda e: e.activation(out=sc[:, :], in_=psR[:, :], func=AF.Sigmoid), (b_psR,), (b_rt,))
                    R_(lambda e: e.tensor_tensor(out=sel[:, :], in0=sc[:, :], in1=rb[:, :], op=ALU.add))
                    v3 = lambda t_: t_[:, :].rearrange("p (g e) -> p g e", e=8)
                    bc3 = lambda col: col.unsqueeze(2).to_broadcast([128, 8, 8])
                    R_(lambda e: e.tensor_reduce(out=sm[:, 0:8], in_=v3(sel), axis=AX.X, op=ALU.max))
                    R_(lambda e: e.tensor_tensor(out=v3(eq), in0=v3(sel), in1=bc3(sm[:, 0:8]), op=ALU.is_equal))
                    R_(lambda e: e.scalar_tensor_tensor(out=tmp64[:, :], in0=eq[:, :], scalar=-BIG, in1=sel[:, :], op0=ALU.mult, op1=ALU.add))
                    R_(lambda e: e.tensor_reduce(out=sm[:, 8:16], in_=v3(tmp64), axis=AX.X, op=ALU.max))
                    R_(lambda e: e.tensor_tensor(out=sm[:, 16:24], in0=sm[:, 0:8], in1=sm[:, 8:16], op=ALU.add))
                    R_(lambda e: e.max(out=sm[:, 24:32], in_=sm[:, 16:24]))
                    R_(lambda e: e.tensor_scalar(out=sm[:, 32:40], in0=sm[:, 16:24], scalar1=sm[:, 27:28], scalar2=None, op0=ALU.is_ge))
                    R_(lambda e: e.tensor_scalar(out=sm[:, 40:48], in0=sm[:, 32:40], scalar1=-1.0, scalar2=BIG, op0=ALU.add, op1=ALU.mult))
                    R_(lambda e: e.tensor_tensor(out=v3(msel), in0=v3(sel), in1=bc3(sm[:, 32:40]), op=ALU.mult))
                    R_(lambda e: e.tensor_tensor(out=v3(msel), in0=v3(msel), in1=bc3(sm[:, 40:48]), op=ALU.add))
                    R_(lambda e: e.max(out=sm[:, 48:56], in_=msel[:, :]))
                    R_(lambda e: e.tensor_scalar(out=Mm[:, :], in0=msel[:, :], scalar1=sm[:, 53:54], scalar2=None, op0=ALU.is_ge))
                    R_(lambda e: e.tensor_tensor(out=tmp64[:, :], in0=sc[:, :], in1=Mm[:, :], op=ALU.mult))
                    R_(lambda e: e.tensor_reduce(out=sm[:, 56:57], in_=tmp64[:, :], axis=AX.X, op=ALU.add))
                    R_(lambda e: e.reciprocal(out=sm[:, 57:58], in_=sm[:, 56:57]))
                    R_(lambda e: e.tensor_scalar(out=Wt[:, :], in0=tmp64[:, :], scalar1=sm[:, 57:58], scalar2=2.5, op0=ALU.mult, op1=ALU.mult))
                    R_(lambda e: e.tensor_copy(out=Mb[:, :], in_=Mm[:, :]))
                    S.op("pe", lambda e: e.matmul(psP[:, 0:NE], lhsT=SLT[:, :], rhs=Mb[:, :], start=True, stop=True), (b_rt, b_c4), (b_psP,))
                    S.op("pe", lambda e: e.matmul(psP[:, NE:2 * NE], lhsT=ONES[:, :], rhs=Mb[:, :], start=True, stop=True), (b_rt, b_c4), (b_psP,))
                    R_(lambda e: e.tensor_tensor(out=pos[:, :], in0=psP[:, 0:NE], in1=cntb[:, :], op=ALU.add), (b_psP, b_cnt))
                    S.op("dve", lambda e: e.tensor_tensor(out=cntb[:, :], in0=cntb[:, :], in1=psP[:, NE:2 * NE], op=ALU.add), (b_psP, b_cnt, b_rt), (b_cnt,))
                    R_(lambda e: e.tensor_scalar(out=okM[:, :], in0=pos[:, :], scalar1=float(CAP), scalar2=None, op0=ALU.is_lt))
                    R_(lambda e: e.tensor_tensor(out=okM[:, :], in0=okM[:, :], in1=Mm[:, :], op=ALU.mult))
                    R_(lambda e: e.tensor_tensor(out=slotf[:, :], in0=pos[:, :], in1=eoff[:, :], op=ALU.add))
                    R_(lambda e: e.tensor_tensor(out=slotf[:, :], in0=slotf[:, :], in1=okM[:, :], op=ALU.mult))
                    R_(lambda e: e.tensor_tensor(out=Wt[:, :], in0=Wt[:, :], in1=okM[:, :], op=ALU.mult))
                    R_(lambda e: e.memset(slot_f[:, :], 0.0))
                    R_(lambda e: e.memset(scat_f[:, :], 0.0), (), (b_scat,))
                    for k in range(6):
                        R_(lambda e, k=k: e.tensor_scalar(out=eq[:, :], in0=msel[:, :], scalar1=sm[:, 48 + k:49 + k], scalar2=None, op0=ALU.is_equal))
                        R_(lambda e: e.tensor_tensor(out=tmp64[:, :], in0=eq[:, :], in1=slotf[:, :], op=ALU.mult))
                        R_(lambda e, k=k: e.tensor_reduce(out=slot_f[:, k:k + 1], in_=tmp64[:, :], axis=AX.X, op=ALU.add))
                        R_(lambda e: e.tensor_tensor(out=tmp64[:, :], in0=eq[:, :], in1=Wt[:, :], op=ALU.mult))
                        R_(lambda e, k=k, ti=ti: e.tensor_reduce(out=w_all[:, ti, k:k + 1], in_=tmp64[:, :], axis=AX.X, op=ALU.add), (), (b_tab,))
                        R_(lambda e: e.tensor_tensor(out=tmp64[:, :], in0=eq[:, :], in1=okM[:, :], op=ALU.mult))
                        R_(lambda e, k=k: e.tensor_reduce(out=sm[:, 58 + k:59 + k], in_=tmp64[:, :], axis=AX.X, op=ALU.add))
                    R_(lambda e: e.tensor_scalar(out=scat_f[:, 0:6], in0=sm[:, 58:64], scalar1=-1.0, scalar2=-1.0e6, op0=ALU.add, op1=ALU.mult), (), (b_scat,))
                    R_(lambda e: e.tensor_tensor(out=scat_f[:, 0:6], in0=scat_f[:, 0:6], in1=slot_f[:, 0:6], op=ALU.add), (b_scat,), (b_scat,))
                    R_(lambda e: e.tensor_scalar(out=scat_f[:, :], in0=scat_f[:, :], scalar1=0.0, scalar2=2.0e6, op0=ALU.max, op1=ALU.min), (b_scat,), (b_scat,))
                    R_(lambda e: e.tensor_scalar(out=slot_f[:, :], in0=slot_f[:, :], scalar1=0.0, scalar2=float(NSLOT - 1), op0=ALU.max, op1=ALU.min))
                    R_(lambda e: e.tensor_copy(out=scat_i[:, :], in_=scat_f[:, :]), (b_scat,), (b_scat,))
                    R_(lambda e, ti=ti: e.tensor_copy(out=slot_i[:, ti, :], in_=slot_f[:, :]), (), (b_tab,))
                    for k in range(6 if not os.environ.get("NOSCAT") else 0):
                        def scat_fn(e, k=k, ti=ti):
                            if "bc" not in regcache:
                                regcache["bc"] = e.to_reg(NSLOT - 1)
                            return e.indirect_dma_start(
                                out=slot_tok[:, :], out_offset=bass.IndirectOffsetOnAxis(ap=scat_i[:, k:k + 1], axis=0),
                                in_=tokid[:, ti:ti + 1], in_offset=None, bounds_check=regcache["bc"], oob_is_err=False)
                        S.op("pool", scat_fn,
                            (b_scat, b_c4), (b_slot_tok,), dma=True, waw=(ti == 0 and k == 0))
                    if ti == 0:
                        dbg("Mm", Mm[:, :], b_rt, [128, NE]); dbg("Wt", Wt[:, :], b_rt, [128, NE]); dbg("sc", sc[:, :], b_rt, [128, NE])
                        dbg("sm", sm[:, :], b_rt, [128, 64]); dbg("pos", pos[:, :], b_rt, [128, NE])
                dbg("slot_i", slot_i[:, :, :], b_tab, [128, nt, 8], I32); dbg("w_all", w_all[:, :, :], b_tab, [128, nt, 8])
            finals += [b_x1, b_xn2, b_slot_tok]

        ybh = [nc.dram_tensor("yb%d" % i, [NSLOT, D // 2], F32, kind="Internal").ap() for i in range(2)]
        b_ysh = B("ysh"); b_yb = B("yb")
        if phases >= 5:
            S.barrier()
            with ExitStack() as ph:
                psb = lambda name, shape, dt=F32: ph.enter_context(nc.sbuf_tensor(name, list(shape), dt))
                pps = lambda name, shape, dt=F32: ph.enter_context(nc.psum_tensor(name, list(shape), dt))
                gath = [psb("gath%d" % i, [128, D], BF16) for i in range(2)]; b_gath = [B("gath%d" % i) for i in range(2)]
                idxs = [psb("idxs%d" % i, [128, 4], I32) for i in range(2)]; b_idx = [B("idxs%d" % i) for i in range(2)]
                xT = [psb("xT%d" % i, [128, NK, 512], BF16) for i in range(2)]; b_xT = [B("xT%d" % i) for i in range(2)]
                NWG = 4
                WgS = [psb("WgS%d" % i, [128, NK, 256], BF16) for i in range(NWG)]; b_Wg = [B("WgS%d" % i) for i in range(NWG)]
                NWD = 3
                WdS = [psb("WdS%d" % i, [128, NF, 512], BF16) for i in range(NWD)]; b_Wd = [B("WdS%d" % i) for i in range(NWD)]
                hidT = [psb("hidT%d" % i, [128, NF, 512], BF16) for i in range(2)]; b_hid = [B("hidT%d" % i) for i in range(2)]
                sg = [psb("sg%d" % i, [128, 512]) for i in range(2)]; b_sg = [B("sg%d" % i) for i in range(2)]
                stg = [psb("stg5_%d" % i, [128, 512]) for i in range(4)]; b_stg = [B("stg5_%d" % i) for i in range(4)]
                psT = [pps("psT5_%d" % i, [128, 1024], BF16) for i in range(2)]; b_psT = [B("psT5_%d" % i) for i in range(2)]
                pg_ = [pps("pg%d" % i, [128, 512]) for i in range(2)]; b_pg = [B("pg%d" % i) for i in range(2)]
                pu_ = [pps("pu%d" % i, [128, 512]) for i in range(2)]; b_pu = [B("pu%d" % i) for i in range(2)]
                po_ = [pps("po%d" % i, [128, 512]) for i in range(2)]; b_po = [B("po%d" % i) for i in range(2)]
                SC = min(512, T)
                items = [("s", c) for c in range(T // SC)] + [("r", e) for e in range(NE)]
                if os.environ.get("NEXP"):
                    items = items[:T // SC + int(os.environ["NEXP"])]
                cn = {"g": 0, "wg": 0, "wd": 0, "st": 0, "f": 0, "o": 0}
                def item_ctx(it):
                    kind, e = items[it]
                    xs_ = it % 2
                    ncols = SC if kind == "s" else CAP
                    nblk = ncols // 128
                    hs = it % 2
                    return kind, e, xs_, ncols, nblk, hs

                def wviews(kind, e):
                    if kind == "s":
                        wg_v = I("ws_gate").rearrange("(k p) f -> p k f", p=128)
                        wu_v = I("ws_up").rearrange("(k p) f -> p k f", p=128)
                        wd_v = I("ws_down").rearrange("(f p) n -> p f n", p=128)
                    else:
                        wg_v = I("we_gate")[e].rearrange("(k p) f -> p k f", p=128)
                        wu_v = I("we_up")[e].rearrange("(k p) f -> p k f", p=128)
                        wd_v = I("we_down")[e].rearrange("(f p) n -> p f n", p=128)
                    return wg_v, wu_v, wd_v

                def stageA(it):
                    kind, e, xs_, ncols, nblk, hs = item_ctx(it)
                    for blk in range(nblk):
                        gs = cn["g"] % 2
                        cn["g"] += 1
                        if kind == "s":
                            r0 = e * SC + blk * 128
                            dma("sp", gath[gs][:, :], xn2[r0:r0 + 128, :], (b_xn2,), (b_gath[gs],))
                        else:
                            r0 = e * CAP + blk * 128
                            dma("sp", idxs[gs][:, 0:1], slot_tok[r0:r0 + 128, 0:1], (b_slot_tok,), (b_idx[gs],))
                            S.op("pool", (lambda ee, gs=gs: ee.indirect_dma_start(
                                out=gath[gs][:, :], out_offset=None, in_=xn2[:, :],
                                in_offset=bass.IndirectOffsetOnAxis(ap=idxs[gs][:, 0:1], axis=0))),
                                (b_idx[gs], b_xn2), (b_gath[gs],), dma=True)
                        aprep_transposes(psT, b_psT, gath[gs], b_gath[gs], xT[xs_], b_xT[xs_], blk)

                def stageB(it):
                    kind, e, xs_, ncols, nblk, hs = item_ctx(it)
                    wg_v, wu_v, wd_v = wviews(kind, e)
                    for fp in range(NF // 2):
                        wgs = cn["wg"] % NWG; cn["wg"] += 1
                        wus = cn["wg"] % NWG; cn["wg"] += 1
                        dma("pool", WgS[wgs][:, :, :], wg_v[:, :, fp * 256:(fp + 1) * 256], (), (b_Wg[wgs],))
                        dma("pool", WgS[wus][:, :, :], wu_v[:, :, fp * 256:(fp + 1) * 256], (), (b_Wg[wus],))
                        for fc in range(2):
                            f = fp * 2 + fc
                            p_ = cn["f"] % 2; cn["f"] += 1
                            for k in range(NK):
                                S.op("pe", (lambda ee, p_=p_, wgs=wgs, k=k, fc=fc, xs_=xs_, ncols=ncols: ee.matmul(
                                    pg_[p_][:, 0:ncols], lhsT=WgS[wgs][:, k, fc * 128:(fc + 1) * 128], rhs=xT[xs_][:, k, 0:ncols],
                                    start=(k == 0), stop=(k == NK - 1))), (b_Wg[wgs], b_xT[xs_]), (b_pg[p_],))
                            for k in range(NK):
                                S.op("pe", (lambda ee, p_=p_, wus=wus, k=k, fc=fc, xs_=xs_, ncols=ncols: ee.matmul(
                                    pu_[p_][:, 0:ncols], lhsT=WgS[wus][:, k, fc * 128:(fc + 1) * 128], rhs=xT[xs_][:, k, 0:ncols],
                                    start=(k == 0), stop=(k == NK - 1))), (b_Wg[wus], b_xT[xs_]), (b_pu[p_],))
                            S.op("act", (lambda ee, p_=p_, ncols=ncols: ee.activation(out=sg[p_][:, 0:ncols], in_=pg_[p_][:, 0:ncols], func=AF.Silu)),
                                 (b_pg[p_],), (b_sg[p_],))
                            S.op("dve", (lambda ee, p_=p_, ncols=ncols, hs=hs, f=f: ee.tensor_tensor(out=hidT[hs][:, f, 0:ncols], in0=sg[p_][:, 0:ncols],
                                                                                                in1=pu_[p_][:, 0:ncols], op=ALU.mult)),
                                 (b_sg[p_], b_pu[p_]), (b_hid[hs],))

                def stageC(it):
                    kind, e, xs_, ncols, nblk, hs = item_ctx(it)
                    wg_v, wu_v, wd_v = wviews(kind, e)
                    for n in range(D // 512):
                        wds = cn["wd"] % NWD; cn["wd"] += 1
                        dma("pool", WdS[wds][:, :, :], wd_v[:, :, n * 512:(n + 1) * 512], (), (b_Wd[wds],))
                        for blk in range(nblk):
                            o_ = cn["o"] % 2; cn["o"] += 1
                            q_ = cn["st"] % 4; cn["st"] += 1
                            for f in range(NF):
                                S.op("pe", (lambda ee, o_=o_, hs=hs, f=f, blk=blk, wds=wds: ee.matmul(
                                    po_[o_][:, :], lhsT=hidT[hs][:, f, blk * 128:(blk + 1) * 128], rhs=WdS[wds][:, f, :],
                                    start=(f == 0), stop=(f == NF - 1))), (b_hid[hs], b_Wd[wds]), (b_po[o_],))
                            if cn["o"] % 2 == 0:
                                S.op("act", (lambda ee, o_=o_, q_=q_: ee.activation(out=stg[q_][:, :], in_=po_[o_][:, :], func=AF.Copy)), (b_po[o_],), (b_stg[q_],))
                            else:
                                S.op("dve", (lambda ee, o_=o_, q_=q_: ee.tensor_copy(out=stg[q_][:, :], in_=po_[o_][:, :])), (b_po[o_],), (b_stg[q_],))
                            if kind == "s":
                                r0 = e * SC + blk * 128
                                dma("sp", ysh[r0:r0 + 128, n * 512:(n + 1) * 512], stg[q_][:, :], (b_stg[q_],), (b_ysh,))
                            else:
                                r0 = e * CAP + blk * 128
                                hh, nn = n // 4, n % 4
                                dma("sp", ybh[hh][r0:r0 + 128, nn * 512:(nn + 1) * 512], stg[q_][:, :], (b_stg[q_],), (b_yb,))

                stageA(0)
                for it in range(len(items)):
                    stageB(it)
                    if it + 1 < len(items):
                        stageA(it + 1)
                    stageC(it)
            finals += [b_ysh, b_yb]

        b_out = B("out")
        if phases >= 6:
            S.barrier()
            with ExitStack() as ph:
                psb = lambda name, shape, dt=F32: ph.enter_context(nc.sbuf_tensor(name, list(shape), dt))
                G2 = psb("G2", [128, D]); b_G2 = B("G2")
                acc = [psb("acc%d" % i, [128, D]) for i in range(2)]; b_acc = [B("acc%d" % i) for i in range(2)]
                x1t = [psb("x1t%d" % i, [128, D]) for i in range(2)]; b_x1t = [B("x1t%d" % i) for i in range(2)]
                gk = [psb("gk%d" % i, [128, D]) for i in range(3)]; b_gk = [B("gk%d" % i) for i in range(3)]
                jk = psb("jk6", [128, D], BF16); b_jk = B("jk6")
                st6 = psb("st6", [128, 4]); b_st6 = [B("st6_%d" % i) for i in range(2)]
                bt = load_gate_bc(psb, G2, gk[0], 5 * D, "g_post_ffn", b_G2)
                b_gk[0].W = list(b_G2.W); b_gk[0].R = list(b_G2.R)
                ng = 0
                for ti in range(nt):
                    s_ = ti % 2
                    rows = slice(ti * 128, ti * 128 + 128)
                    dma("sp", acc[s_][:, :], ysh[rows, :], (b_ysh,), (b_acc[s_],))
                    dma("sp", x1t[s_][:, :], x1[rows, :], (b_x1,), (b_x1t[s_],))
                    for k in range(6):
                        gsl = ng % 3; ng += 1
                        for hh in range(2):
                            S.op("pool", (lambda ee, gsl=gsl, ti=ti, k=k, hh=hh: ee.indirect_dma_start(
                                out=gk[gsl][:, hh * (D // 2):(hh + 1) * (D // 2)], out_offset=None, in_=ybh[hh][:, :],
                                in_offset=bass.IndirectOffsetOnAxis(ap=slot_i[:, ti, k:k + 1], axis=0))),
                                (b_tab, b_yb), (b_gk[gsl],), dma=True)
                        S.op("dve", (lambda ee, gsl=gsl, ti=ti, k=k, s_=s_: ee.scalar_tensor_tensor(
                            out=acc[s_][:, :], in0=gk[gsl][:, :], scalar=w_all[:, ti, k:k + 1], in1=acc[s_][:, :], op0=ALU.mult, op1=ALU.add)),
                            (b_gk[gsl], b_tab, b_acc[s_]), (b_acc[s_],))
                    ssv = st6[:, 2 * s_:2 * s_ + 1]; rsv = st6[:, 2 * s_ + 1:2 * s_ + 2]
                    S.op("dve", (lambda ee, ssv=ssv: ee.memset(ssv, 0.0)), (), (b_st6[s_],))
                    S.op("act", (lambda ee, s_=s_, ssv=ssv: ee.activation(out=jk[:, :], in_=acc[s_][:, :], func=AF.Square, accum_out=ssv)),
                         (b_acc[s_],), (b_jk, b_st6[s_]))
                    rstd(rsv, ssv, D, (b_st6[s_],))
                    S.op("dve", (lambda ee, s_=s_, rsv=rsv: ee.scalar_tensor_tensor(out=acc[s_][:, :], in0=acc[s_][:, :], scalar=rsv, in1=G2[:, :],
                                                                                  op0=ALU.mult, op1=ALU.mult)), (b_acc[s_], b_st6[s_], b_G2), (b_acc[s_],))
                    S.op("dve", (lambda ee, s_=s_: ee.tensor_tensor(out=x1t[s_][:, :], in0=x1t[s_][:, :], in1=acc[s_][:, :], op=ALU.add)),
                         (b_x1t[s_], b_acc[s_]), (b_x1t[s_],))
                    dma("sp", out[rows, :], x1t[s_][:, :], (b_x1t[s_],), (b_out,))
            finals += [b_out]

        finals += dbg_list
        S.emit(final_bufs=finals)
    S.used_inputs = list(used_inputs.keys())
    return nc, S


def _prep_inputs(inp):
    f = lambda a: np.ascontiguousarray(np.asarray(a, dtype=np.float32))
    x = f(inp["x"])
    c = f(inp["c"])
    L = 0
    common = {
        "w_ada": f(inp["w_ada"][L]),
        "b_ada": f(inp["b_ada"][L]).reshape(1, -1),
        "gT_pre_mix": f(inp["g_pre_mix"][L].reshape(NK, 128).T),
        "gT_pre_ffn": f(inp["g_pre_ffn"][L].reshape(NK, 128).T),
        "g_post_mix": f(inp["g_post_mix"][L]).reshape(1, -1),
        "g_pre_ffn": f(inp["g_pre_ffn"][L]).reshape(1, -1),
        "g_post_ffn": f(inp["g_post_ffn"][L]).reshape(1, -1),
        "w_in": f(inp["w_in"][L]),
        "a_ln_g": f(inp["a_ln_g"][L]).reshape(1, -1),
        "a_ln_b": f(inp["a_ln_b"][L]).reshape(1, -1),
        "a_w_s": f(inp["a_w_s"][L]),
        "a_b_sT": f(inp["a_b_s"][L].T),
        "a_out_g": f(inp["a_out_g"][L]).reshape(1, -1),
        "b_w_g2a": f(np.concatenate([inp["b_w_g2"][L], inp["b_b_g2"][L].reshape(1, -1)], axis=0)),
        "b_head_g": f(inp["b_head_g"][L]).reshape(1, -1),
        "w_out": f(inp["w_out"][L]),
        "w_router": f(inp["w_router"][L]),
        "router_bias": f(inp["router_bias"][L]).reshape(1, -1),
        "we_gate": f(inp["we_gate"][L]),
        "we_up": f(inp["we_up"][L]),
        "we_down": f(inp["we_down"][L]),
        "ws_gate": f(inp["ws_gate"][L]),
        "ws_up": f(inp["ws_up"][L]),
        "ws_down": f(inp["ws_down"][L]),
    }
    return x, c, common


def prefix_inputs(xb, q, nt):
    T = nt * 128
    NP = 3 * nt
    xp = np.zeros((NP * 128, D), np.float32)
    pm = np.zeros((128, NP), np.float32)
    n = q * T
    if n:
        xp[NP * 128 - n:] = xb[0:n]
        pm[:, NP - n // 128:] = 1.0
    return xp, pm


def kernel(**inp):
    x, c, common = _prep_inputs(inp)
    nc, S = build(nt=16)
    in_maps = []
    for core in range(NCORES):
        b, q = core // 4, core % 4
        m = dict(common)
        m["x"] = np.ascontiguousarray(x[b, q * 2048:(q + 1) * 2048, :])
        m["cT"] = np.ascontiguousarray(c[b].reshape(NK, 128).T)
        xp, pm = prefix_inputs(x[b], q, 16)
        m["xprev"] = xp
        m["pmask"] = pm
        in_maps.append({k: m[k] for k in S.used_inputs})
    res = run_bass_kernel_spmd(nc, in_maps, core_ids=list(range(NCORES)))
    outs = [r["out"] for r in res.results]
    full = np.stack([np.concatenate(outs[0:4], axis=0), np.concatenate(outs[4:8], axis=0)], axis=0)
    return full.astype(np.float32)
```

```python
import os
import numpy as np
import concourse.bass as bass
import concourse.mybir as mybir
from concourse.bass_utils import run_bass_kernel_spmd
from contextlib import ExitStack

F32 = mybir.dt.float32
BF16 = mybir.dt.bfloat16
I32 = mybir.dt.int32
ALU = mybir.AluOpType
AF = mybir.ActivationFunctionType
AX = mybir.AxisListType

D = 4096
NK = D // 128
DIN = 10256
NMOD = 6
AW = 2048
BKW = 1024
BW = 2048
NE = 64
DE = 768
NF = DE // 128
EPS = 1e-6
CAP = 384
NCORES = 8


class Buf:
    __slots__ = ("name", "W", "R", "nd")

    def __init__(self, name):
        self.name = name
        self.W = []
        self.R = []
        self.nd = 0


class Op:
    __slots__ = ("eng", "fn", "deps", "dma", "key", "ticket", "signal", "hard", "raw")


class Sched:
    ENGS = ("pe", "act", "dve", "pool", "sp")

    def __init__(self, nc):
        self.nc = nc
        self.ops = {e: [] for e in self.ENGS}
        self.nops = 0
        self.bar = {e: set() for e in self.ENGS}
        self.dmas_since = []
        self.mute = False

    def barrier(self):
        deps = set(self.dmas_since)
        for e in self.ENGS:
            for o in reversed(self.ops[e]):
                if not o.dma:
                    deps.add(o)
                    break
        for e in self.ENGS:
            self.bar[e] |= deps
        self.dmas_since = []

    def op(self, eng, fn, reads=(), writes=(), dma=False, hard=False, waw=False):
        if self.mute:
            return None
        o = Op()
        o.hard = hard
        o.eng = eng
        o.fn = fn
        o.dma = dma
        o.key = None
        o.ticket = None
        o.signal = False
        deps = set()
        for b in reads:
            for w in b.W:
                deps.add(w)
        o.raw = set(d for d in deps if (not d.dma) and d.eng == eng and eng != "pe")
        for b in writes:
            for r in b.R:
                deps.add(r)
            for w in b.W:
                if waw or not (dma and w.dma):
                    deps.add(w)
        if self.bar[eng]:
            deps |= self.bar[eng]
            self.bar[eng] = set()
        deps.discard(o)
        o.deps = deps
        for b in reads:
            b.R.append(o)
        for b in writes:
            if b.R:
                b.R = []
                b.W = [o]
            else:
                if b.W and (not dma) and (not b.W[-1].dma) and b.W[-1].eng == eng:
                    b.W[-1] = o
                else:
                    b.W.append(o)
        if dma:
            assert len(writes) >= 1
            o.key = writes[0]
            o.key.nd += 1
            o.ticket = 16 * o.key.nd
            self.dmas_since.append(o)
        self.ops[eng].append(o)
        self.nops += 1
        return o

    def emit(self, final_bufs=()):
        nc = self.nc
        for e in self.ENGS:
            for o in self.ops[e]:
                for d in o.deps:
                    if not d.dma and (d.eng != o.eng or o.hard or d in o.raw):
                        d.signal = True
        for e in self.ENGS:
            n = 0
            for o in self.ops[e]:
                if not o.dma and o.signal:
                    n += 1
                    o.ticket = n
        keys = []
        seen = set()
        for e in self.ENGS:
            for o in self.ops[e]:
                if o.dma and id(o.key) not in seen:
                    seen.add(id(o.key))
                    keys.append(o.key)
        with ExitStack() as st:
            esem = {e: st.enter_context(nc.semaphore("sem_" + e)) for e in self.ENGS}
            ksem = {id(k): st.enter_context(nc.semaphore("sd_%d" % i)) for i, k in enumerate(keys)}
            self.nsem = len(keys) + len(esem)
            block = st.enter_context(nc.Block())
            engobj = {"pe": block.tensor, "act": block.scalar, "dve": block.vector,
                      "pool": block.gpsimd, "sp": block.sync}

            def make(ename):
                ops = self.ops[ename]

                def body(eng):
                    waited = {}
                    for o in ops:
                        need = {}
                        for d in o.deps:
                            if d.dma:
                                s = ksem[id(d.key)]
                                sk = ("k", id(d.key))
                            else:
                                if d.eng == ename and not (o.hard or d in o.raw):
                                    continue
                                s = esem[d.eng]
                                sk = ("e", d.eng)
                            if need.get(sk, (None, 0))[1] < d.ticket:
                                need[sk] = (s, d.ticket)
                        for sk, (s, v) in need.items():
                            if waited.get(sk, 0) < v:
                                eng.wait_ge(s, v)
                                waited[sk] = v
                        ins = o.fn(eng)
                        if o.dma:
                            ins.then_inc(ksem[id(o.key)], 16)
                        elif o.signal:
                            ins.then_inc(esem[ename], 1)
                    if ename == "sp":
                        for b in final_bufs:
                            if b.nd:
                                eng.wait_ge(ksem[id(b)], 16 * b.nd)
                return body

            for ename in self.ENGS:
                engobj[ename](make(ename))


def build(nt=16, phases=99, debug=False):
    T = nt * 128
    G = min(8, nt)
    NG = nt // G
    GT = G * 128
    nc = bass.Bass("TRN2", target_bir_lowering=False)
    S = Sched(nc)

    def din(name, shape, dt=F32):
        return nc.dram_tensor(name, list(shape), dt, kind="ExternalInput").ap()

    def dscr(name, shape, dt=F32, out=False):
        return nc.dram_tensor(name, list(shape), dt,
                              kind="ExternalOutput" if (out or debug) else "Internal").ap()

    INSH = {
        "x": [T, D], "cT": [128, NK], "w_ada": [D, NMOD * D], "b_ada": [1, NMOD * D],
        "gT_pre_mix": [128, NK], "gT_pre_ffn": [128, NK], "g_post_mix": [1, D], "g_post_ffn": [1, D],
        "w_in": [D, DIN], "a_ln_g": [1, AW], "a_ln_b": [1, AW], "a_w_s": [8, 128, 128],
        "a_b_sT": [128, 8], "a_out_g": [1, AW], "b_w_g2a": [17, BKW], "b_head_g": [1, 512],
        "w_out": [D, D], "w_router": [D, NE], "router_bias": [1, NE],
        "we_gate": [NE, D, DE], "we_up": [NE, D, DE], "we_down": [NE, DE, D],
        "ws_gate": [D, DE], "ws_up": [D, DE], "ws_down": [DE, D],
        "xprev": [3 * T, D], "pmask": [128, 3 * nt], "g_pre_ffn": [1, D], "g_pre_mix": [1, D],
    }
    used_inputs = {}

    def I(name):
        if name not in used_inputs:
            used_inputs[name] = nc.dram_tensor(name, list(INSH[name]), F32, kind="ExternalInput").ap()
        return used_inputs[name]

    out = dscr("out", [T, D], out=True)

    modrow = dscr("modrow", [1, NMOD * D])
    pu = dscr("p_u", [T, AW])
    pva = dscr("p_va", [T, AW])
    pq = dscr("p_q", [T, BKW])
    pk = dscr("p_k", [T, BKW])
    pvb = dscr("p_vb", [T, BW])
    pr = dscr("p_r", [T, BW])
    pg = dscr("p_g", [T, 16])
    NP = 3 * nt
    ppk = dscr("pp_k", [NP * 128, BKW])
    ppv = dscr("pp_vb", [NP * 128, BW])
    ppg = dscr("pp_g", [NP * 128, 16])
    ycat = dscr("ycat", [T, D], BF16)
    yraw = dscr("yraw", [T, D])
    x1 = dscr("x1", [T, D])
    xn2 = dscr("xn2", [T, D], BF16)
    ysh = dscr("ysh", [T, D])

    B = Buf
    b_modrow = B("modrow")

    def dma(eng, out_ap, in_ap, reads, writes):
        return S.op(eng, lambda e: e.dma_start(out=out_ap, in_=in_ap), reads, writes, dma=True)

    dbg_list = []
    regcache = {}

    def dbg(name, ap, buf, shape, dt=F32):
        if not debug:
            return
        t_ = nc.dram_tensor("dbg_" + name, list(shape), dt, kind="ExternalOutput").ap()
        if not dbg_list:
            dbg_list.append(B("dbg_all"))
        dma("sp", t_, ap, (buf,), (dbg_list[0],))

    with ExitStack() as top:
        sb = lambda name, shape, dt=F32: top.enter_context(nc.sbuf_tensor(name, list(shape), dt))
        ident_bf = sb("ident_bf", [128, 128], BF16)
        ident_f = sb("ident_f", [128, 128], F32)
        b_ident = B("ident")
        S.op("pool", lambda e: e.memset(ident_f[:, :], 0.0), (), (b_ident,))
        S.op("pool", lambda e: e.affine_select(out=ident_f[:, :], in_=ident_f[:, :], pattern=[[-1, 128]],
                                               compare_op=ALU.not_equal, fill=1.0, base=0,
                                               channel_multiplier=1), (b_ident,), (b_ident,))
        S.op("pool", lambda e: e.tensor_copy(out=ident_bf[:, :], in_=ident_f[:, :]), (b_ident,), (b_ident,))
        eps_c = sb("eps_c", [128, 1])
        S.op("pool", lambda e: e.memset(eps_c[:, :], EPS), (), (b_ident,))
        one_c = sb("one_c", [128, 1])
        S.op("pool", lambda e: e.memset(one_c[:, :], 1.0), (), (b_ident,))

        with ExitStack() as ph:
            psb = lambda name, shape, dt=F32: ph.enter_context(nc.sbuf_tensor(name, list(shape), dt))
            c_sb = psb("c_sb", [128, NK])
            c_bf = psb("c_bf", [128, NK], BF16)
            b_c = B("c")
            dma("sp", c_sb[:, :], I("cT")[:, :], (), (b_c,))
            S.op("act", lambda e: e.activation(out=c_bf[:, :], in_=c_sb[:, :], func=AF.Silu), (b_c,), (b_c,))
            NB0 = NMOD * D // 512
            wsl = [psb("w0_%d" % i, [128, NK, 512], BF16) for i in range(2)]
            b_w = [B("w0_%d" % i) for i in range(2)]
            bsl = [psb("b0_%d" % i, [1, 512]) for i in range(2)]
            b_b = [B("b0_%d" % i) for i in range(2)]
            msl = [psb("m0_%d" % i, [1, 512]) for i in range(2)]
            b_m = [B("m0_%d" % i) for i in range(2)]
            ps0 = [ph.enter_context(nc.psum_tensor("ps0_%d" % i, [128, 512], F32)) for i in range(2)]
            b_ps = [B("ps0_%d" % i) for i in range(2)]
            w_ada_v = I("w_ada").rearrange("(k p) n -> p k n", p=128)
            for blk in range(NB0):
                s_ = blk % 2
                cs = slice(blk * 512, (blk + 1) * 512)
                dma("pool", wsl[s_][:, :, :], w_ada_v[:, :, cs], (), (b_w[s_],))
                dma("sp", bsl[s_][:, :], I("b_ada")[0:1, cs], (), (b_b[s_],))
                for k in range(NK):
                    S.op("pe", (lambda e, s_=s_, k=k: e.matmul(ps0[s_][0:1, :], lhsT=c_bf[:, k:k + 1],
                                                                rhs=wsl[s_][:, k, :], start=(k == 0),
                                                                stop=(k == NK - 1))),
                         (b_c, b_w[s_]), (b_ps[s_],))
                S.op("dve", (lambda e, s_=s_: e.tensor_tensor(out=msl[s_][:, :], in0=ps0[s_][0:1, :],
                                                               in1=bsl[s_][:, :], op=ALU.add)),
                     (b_ps[s_], b_b[s_]), (b_m[s_],))
                dma("sp", modrow[0:1, cs], msl[s_][:, :], (b_m[s_],), (b_modrow,))

        S.barrier()
        finals = [b_modrow]
        RSQ = lambda dst, src_, n: None

        def rstd(dst, ssum, n, bufs):
            S.op("act", (lambda e: e.activation(out=dst, in_=ssum, func=AF.Sqrt, bias=eps_c[:, 0:1], scale=1.0 / n)),
                 tuple(bufs) + (b_ident,), tuple(bufs))
            S.op("dve", (lambda e: e.reciprocal(out=dst, in_=dst)), tuple(bufs), tuple(bufs))

        def load_modT(psb, name, col0, gname):
            shT = psb(name + "_shT", [128, NK])
            scT = psb(name + "_scT", [128, NK])
            gT = psb(name + "_gT", [128, NK])
            A = psb(name + "_A", [128, NK])
            bm = B(name + "_mod")
            S.op("sp", lambda e: e.dma_start(out=shT[:, :], in_=modrow[0:1, col0:col0 + D].rearrange("o (k p) -> p (o k)", p=128),
                                             allow_slow_non_contiguous=True), (b_modrow,), (bm,), dma=True)
            S.op("sp", lambda e: e.dma_start(out=scT[:, :], in_=modrow[0:1, col0 + D:col0 + 2 * D].rearrange("o (k p) -> p (o k)", p=128),
                                             allow_slow_non_contiguous=True), (b_modrow,), (bm,), dma=True)
            dma("sp", gT[:, :], I(gname)[:, :], (), (bm,))
            S.op("dve", lambda e: e.scalar_tensor_tensor(out=A[:, :], in0=scT[:, :], scalar=1.0, in1=gT[:, :],
                                                         op0=ALU.add, op1=ALU.mult), (bm,), (bm,))
            return A, shT, bm

        def make_gemm(ph, psb, name, nslots=2, nst=4):
            wsl = [psb(name + "_w%d" % i, [128, NK, 512], BF16) for i in range(nslots)]
            b_w = [B(name + "_w%d" % i) for i in range(nslots)]
            NST = nst
            stg = [psb(name + "_st%d" % i, [128, 512]) for i in range(NST)]
            b_st = [B(name + "_st%d" % i) for i in range(NST)]
            NPS = 4
            pacc = [ph.enter_context(nc.psum_tensor(name + "_pa%d" % i, [128, 512], F32)) for i in range(NPS)]
            b_pa = [B(name + "_pa%d" % i) for i in range(NPS)]
            state = {"cnt": 0, "blk": 0}

            def run(hT, b_hT, ntile, w_view, blocks, store):
                for bi, (c0, wd) in enumerate(blocks):
                    s_ = state["blk"] % nslots
                    state["blk"] += 1
                    dma("pool", wsl[s_][:, :, 0:wd], w_view[:, :, c0:c0 + wd], (), (b_w[s_],))
                    for t in range(ntile):
                        cnt = state["cnt"]
                        p_ = cnt % NPS
                        q_ = cnt % NST
                        for k in range(NK):
                            S.op("pe", (lambda e, p_=p_, s_=s_, k=k, t=t, wd=wd: e.matmul(
                                pacc[p_][:, 0:wd], lhsT=hT[:, k, t * 128:(t + 1) * 128], rhs=wsl[s_][:, k, 0:wd],
                                start=(k == 0), stop=(k == NK - 1))), (b_hT[t], b_w[s_]), (b_pa[p_],))
                        if cnt % 2 == 0:
                            S.op("act", (lambda e, p_=p_, q_=q_, wd=wd: e.activation(out=stg[q_][:, 0:wd], in_=pacc[p_][:, 0:wd], func=AF.Copy)),
                                 (b_pa[p_],), (b_st[q_],))
                        else:
                            S.op("dve", (lambda e, p_=p_, q_=q_, wd=wd: e.tensor_copy(out=stg[q_][:, 0:wd], in_=pacc[p_][:, 0:wd])),
                                 (b_pa[p_],), (b_st[q_],))
                        store(t, bi, stg[q_], b_st[q_])
                        state["cnt"] += 1
            return run

        def aprep_transposes(psT, b_psT, src_bf, b_src, hT, b_hTt, t, A=None, Bc=None, b_mod=None):
            for c4 in range(NK // 8):
                p_ = c4 % 2
                for j in range(8):
                    c = c4 * 8 + j
                    S.op("pe", (lambda e, p_=p_, j=j, c=c: e.transpose(out=psT[p_][:, j * 128:(j + 1) * 128],
                                                                     in_=src_bf[:, c * 128:(c + 1) * 128], identity=ident_bf[:, :])),
                         (b_src, b_ident), (b_psT[p_],))
                if A is None:
                    eng = "act" if c4 % 2 == 0 else "dve"
                    dst = hT[:, c4 * 8:(c4 + 1) * 8, t * 128:(t + 1) * 128]
                    srcv = psT[p_][:, :].rearrange("p (j n) -> p j n", n=128)
                    if eng == "act":
                        S.op("act", (lambda e, dst=dst, srcv=srcv: e.activation(out=dst, in_=srcv, func=AF.Copy)), (b_psT[p_],), (b_hTt,))
                    else:
                        S.op("dve", (lambda e, dst=dst, srcv=srcv: e.tensor_copy(out=dst, in_=srcv)), (b_psT[p_],), (b_hTt,))
                else:
                    for j in range(8):
                        c = c4 * 8 + j
                        dst = hT[:, c, t * 128:(t + 1) * 128]
                        srcv = psT[p_][:, j * 128:(j + 1) * 128]
                        if j % 2 == 0:
                            S.op("act", (lambda e, dst=dst, srcv=srcv, c=c: e.activation(out=dst, in_=srcv, func=AF.Identity,
                                                                                       bias=Bc[:, c:c + 1], scale=A[:, c:c + 1])),
                                 (b_psT[p_], b_mod), (b_hTt,))
                        else:
                            S.op("dve", (lambda e, dst=dst, srcv=srcv, c=c: e.tensor_scalar(out=dst, in0=srcv, scalar1=A[:, c:c + 1],
                                                                                          scalar2=Bc[:, c:c + 1], op0=ALU.mult, op1=ALU.add)),
                                 (b_psT[p_], b_mod), (b_hTt,))

        def load_gate_bc(psb, dst, tmp, col0, gname, bbuf, add_one=False):
            dma("sp", dst[:, :], modrow[0:1, col0:col0 + D].to_broadcast([128, D]), (b_modrow,), (bbuf,))
            if add_one:
                S.op("dve", lambda e: e.tensor_scalar(out=dst[:, :], in0=dst[:, :], scalar1=1.0, scalar2=None, op0=ALU.add), (bbuf,), (bbuf,))
            bt = B("gbc_tmp")
            dma("sp", tmp[:, :], I(gname)[0:1, :].to_broadcast([128, D]), (), (bt,))
            S.op("pool", lambda e: e.tensor_tensor(out=dst[:, :], in0=dst[:, :], in1=tmp[:, :], op=ALU.mult), (bbuf, bt), (bbuf,))
            return bt

        b_proj = [B("proj%d" % g) for g in range(NG)]
        b_pp = B("pp")
        roles = [(pu, 0, AW), (pva, AW, AW), (pq, 2 * AW, BKW), (pk, 2 * AW + BKW, BKW),
                 (pvb, 2 * AW + 2 * BKW, BW), (pr, 2 * AW + 2 * BKW + BW, BW), (pg, 2 * AW + 2 * BKW + 2 * BW, 16)]
        blocks1 = []
        for (arr, c0, wd) in roles:
            for o in range(0, wd, 512):
                blocks1.append((c0 + o, min(512, wd - o), arr, o))
        if phases >= 1:
            with ExitStack() as ph:
                psb = lambda name, shape, dt=F32: ph.enter_context(nc.sbuf_tensor(name, list(shape), dt))
                hT = psb("hT1", [128, NK, GT], BF16)
                xs = [psb("xs%d" % i, [128, D]) for i in range(2)]
                b_xs = [B("xs%d" % i) for i in range(2)]
                xn1 = psb("xn1", [128, D], BF16)
                xn = [xn1, xn1]
                b_xn1 = B("xn1")
                b_xn = [b_xn1, b_xn1]
                A1bc = psb("A1bc", [128, D]); B1bc = psb("B1bc", [128, D]); b_m1bc = B("m1bc")
                load_gate_bc(psb, A1bc, xs[1], D, "g_pre_mix", b_m1bc, add_one=True)
                b_xs[1].W = list(b_m1bc.W); b_xs[1].R = list(b_m1bc.R)
                dma("sp", B1bc[:, :], modrow[0:1, 0:D].to_broadcast([128, D]), (b_modrow,), (b_m1bc,))
                st1 = psb("st1", [128, 4])
                ss8 = psb("ss8", [128, 16])
                sqj = psb("sqj", [128, 512], BF16); b_sqj = B("sqj")
                b_s1 = [B("st1_%d" % i) for i in range(2)]
                psT = [ph.enter_context(nc.psum_tensor("psT1_%d" % i, [128, 1024], BF16)) for i in range(2)]
                b_psT = [B("psT1_%d" % i) for i in range(2)]
                w_in_v = I("w_in").rearrange("(k p) n -> p k n", p=128)
                gemm1 = make_gemm(ph, psb, "g1", nst=2)
                for g in range(NG):
                    b_hT = [B("hT1_%d_%d" % (g, t)) for t in range(G)]
                    for t in range(G):
                        s_ = t % 2
                        r0 = (g * G + t) * 128
                        dma("sp", xs[s_][:, :], I("x")[r0:r0 + 128, :], (), (b_xs[s_],))
                        ssv = st1[:, 2 * s_:2 * s_ + 1]
                        rsv = st1[:, 2 * s_ + 1:2 * s_ + 2]
                        S.op("dve", (lambda e, s_=s_: e.memset(ss8[:, 8 * s_:8 * s_ + 8], 0.0)), (), (b_s1[s_],))
                        for j8 in range(8):
                            S.op("act", (lambda e, s_=s_, j8=j8: e.activation(out=sqj[:, :], in_=xs[s_][:, j8 * 512:(j8 + 1) * 512], func=AF.Square,
                                                                            accum_out=ss8[:, 8 * s_ + j8:8 * s_ + j8 + 1])),
                                 (b_xs[s_],), (b_sqj, b_s1[s_]))
                        S.op("dve", (lambda e, s_=s_, ssv=ssv: e.tensor_reduce(out=ssv, in_=ss8[:, 8 * s_:8 * s_ + 8], axis=AX.X, op=ALU.add)),
                             (b_s1[s_],), (b_s1[s_],))
                        rstd(rsv, ssv, D, (b_s1[s_],))
                        S.op("act", (lambda e, s_=s_, rsv=rsv: e.activation(out=xs[s_][:, :], in_=xs[s_][:, :], func=AF.Copy, scale=rsv)),
                             (b_xs[s_], b_s1[s_]), (b_xs[s_],))
                        S.op("dve", (lambda e, s_=s_: e.tensor_tensor(out=xs[s_][:, :], in0=xs[s_][:, :], in1=A1bc[:, :], op=ALU.mult)),
                             (b_xs[s_], b_m1bc), (b_xs[s_],))
                        S.op("dve", (lambda e, s_=s_: e.tensor_tensor(out=xn[s_][:, :], in0=xs[s_][:, :], in1=B1bc[:, :], op=ALU.add)),
                             (b_xs[s_], b_m1bc), (b_xn[s_],))
                        aprep_transposes(psT, b_psT, xn[s_], b_xn[s_], hT, b_hT[t], t)

                    def store1(t, bi, stage, b_stage, g=g):
                        c0, wd, arr, o = blocks1[bi]
                        r0 = (g * G + t) * 128
                        dma("sp", arr[r0:r0 + 128, o:o + wd], stage[:, 0:wd], (b_stage,), (b_proj[g],))
                    gemm1(hT, b_hT, G, w_in_v, [(b[0], b[1]) for b in blocks1], store1)
                blocksP = [b for b in blocks1 if b[2] in (pk, pvb, pg)]
                parr = {id(pk): ppk, id(pvb): ppv, id(pg): ppg}
                for gp in range(NP // G):
                    b_hT = [B("hTp_%d_%d" % (gp, t)) for t in range(G)]
                    for t in range(G):
                        s_ = t % 2
                        r0 = (gp * G + t) * 128
                        dma("sp", xs[s_][:, :], I("xprev")[r0:r0 + 128, :], (), (b_xs[s_],))
                        ssv = st1[:, 2 * s_:2 * s_ + 1]
                        rsv = st1[:, 2 * s_ + 1:2 * s_ + 2]
                        S.op("dve", (lambda e, s_=s_: e.memset(ss8[:, 8 * s_:8 * s_ + 8], 0.0)), (), (b_s1[s_],))
                        for j8 in range(8):
                            S.op("act", (lambda e, s_=s_, j8=j8: e.activation(out=sqj[:, :], in_=xs[s_][:, j8 * 512:(j8 + 1) * 512], func=AF.Square,
                                                                            accum_out=ss8[:, 8 * s_ + j8:8 * s_ + j8 + 1])),
                                 (b_xs[s_],), (b_sqj, b_s1[s_]))
                        S.op("dve", (lambda e, s_=s_, ssv=ssv: e.tensor_reduce(out=ssv, in_=ss8[:, 8 * s_:8 * s_ + 8], axis=AX.X, op=ALU.add)),
                             (b_s1[s_],), (b_s1[s_],))
                        rstd(rsv, ssv, D, (b_s1[s_],))
                        S.op("act", (lambda e, s_=s_, rsv=rsv: e.activation(out=xs[s_][:, :], in_=xs[s_][:, :], func=AF.Copy, scale=rsv)),
                             (b_xs[s_], b_s1[s_]), (b_xs[s_],))
                        S.op("dve", (lambda e, s_=s_: e.tensor_tensor(out=xs[s_][:, :], in0=xs[s_][:, :], in1=A1bc[:, :], op=ALU.mult)),
                             (b_xs[s_], b_m1bc), (b_xs[s_],))
                        S.op("dve", (lambda e, s_=s_: e.tensor_tensor(out=xn[s_][:, :], in0=xs[s_][:, :], in1=B1bc[:, :], op=ALU.add)),
                             (b_xs[s_], b_m1bc), (b_xn[s_],))
                        aprep_transposes(psT, b_psT, xn[s_], b_xn[s_], hT, b_hT[t], t)

                    def storeP(t, bi, stage, b_stage, gp=gp):
                        c0, wd, arr, o = blocksP[bi]
                        r0 = (gp * G + t) * 128
                        dma("sp", parr[id(arr)][r0:r0 + 128, o:o + wd], stage[:, 0:wd], (b_stage,), (b_pp,))
                    gemm1(hT, b_hT, G, w_in_v, [(b[0], b[1]) for b in blocksP], storeP)
            finals += b_proj + [b_pp]

        b_ycat = [B("ycat%d" % g) for g in range(NG)]
        if phases >= 2:
            S.barrier()
            with ExitStack() as ph:
                psb = lambda name, shape, dt=F32: ph.enter_context(nc.sbuf_tensor(name, list(shape), dt))
                pps = lambda name, shape, dt=F32: ph.enter_context(nc.psum_tensor(name, list(shape), dt))
                b_c2 = B("c2")
                lng = psb("lng", [128, AW]); lnb = psb("lnb", [128, AW]); aog = psb("aog", [128, AW]); hgb = psb("hgb", [128, 512])
                dma("sp", lng[:, :], I("a_ln_g")[0:1, :].to_broadcast([128, AW]), (), (b_c2,))
                dma("sp", lnb[:, :], I("a_ln_b")[0:1, :].to_broadcast([128, AW]), (), (b_c2,))
                dma("sp", aog[:, :], I("a_out_g")[0:1, :].to_broadcast([128, AW]), (), (b_c2,))
                dma("sp", hgb[:, :], I("b_head_g")[0:1, :].to_broadcast([128, 512]), (), (b_c2,))
                bsT = psb("bsT", [128, 8])
                dma("sp", bsT[:, :], I("a_b_sT")[:, :], (), (b_c2,))
                wg2 = psb("wg2", [32, BKW])
                dma("sp", wg2[0:17, :], I("b_w_g2a")[:, :], (), (b_c2,))
                wsf = psb("wsf", [128, 8, 128])
                dma("sp", wsf[:, :, :], I("a_w_s").rearrange("h i j -> i h j"), (), (b_c2,))
                wsb = psb("wsb", [128, 8, 128], BF16)
                WT = psb("WT", [128, 8, 128], BF16)
                Um = psb("Um", [128, 128]); Lm = psb("Lm", [128, 128]); Mk = psb("Mk", [128, 128])
                negc = psb("negc", [128, 1])
                glT = psb("glT", [32, 128])
                Sf = psb("Sf", [128, 8, 512]); Sb = psb("Sb", [128, 8, 512], BF16)
                b_S = B("S")
                ISC = -1.0 / 16.0
                for h in range(8):
                    S.op("pool", (lambda e, h=h: e.affine_select(out=wsf[:, h, :], in_=wsf[:, h, :], pattern=[[-1, 128]],
                                                                 compare_op=ALU.is_ge, fill=0.0, base=0, channel_multiplier=1)),
                         (b_c2,), (b_c2,))
                S.op("pool", lambda e: e.tensor_copy(out=wsb[:, :, :], in_=wsf[:, :, :]), (b_c2,), (b_c2,))
                S.op("pool", lambda e: e.memset(Um[:, :], ISC), (), (b_c2,))
                S.op("pool", lambda e: e.affine_select(out=Um[:, :], in_=Um[:, :], pattern=[[1, 128]], compare_op=ALU.is_ge,
                                                       fill=0.0, base=0, channel_multiplier=-1), (b_c2,), (b_c2,))
                S.op("pool", lambda e: e.memset(Lm[:, :], ISC), (), (b_c2,))
                S.op("pool", lambda e: e.affine_select(out=Lm[:, :], in_=Lm[:, :], pattern=[[-1, 128]], compare_op=ALU.is_gt,
                                                       fill=0.0, base=0, channel_multiplier=1), (b_c2,), (b_c2,))
                S.op("pool", lambda e: e.memset(Mk[:, :], 1.0), (), (b_c2,))
                S.op("pool", lambda e: e.affine_select(out=Mk[:, :], in_=Mk[:, :], pattern=[[1, 128]], compare_op=ALU.is_ge,
                                                       fill=0.0, base=0, channel_multiplier=-1), (b_c2,), (b_c2,))
                S.op("pool", lambda e: e.memset(negc[:, :], ISC), (), (b_c2,))
                S.op("pool", lambda e: e.memset(glT[:, :], 1.0), (), (b_c2,))
                S.op("pool", lambda e: e.memset(Sf[:, :, :], 0.0), (), (b_S,))
                S.op("pool", lambda e: e.memset(Sb[:, :, :], 0.0), (), (b_S,))
                pA = [pps("pA%d" % i, [128, 512]) for i in range(2)]; b_pA = [B("pA%d" % i) for i in range(2)]
                P1 = pps("P1", [128, 1024]); b_P1 = B("P1")
                P2 = pps("P2", [128, 1024]); b_P2 = B("P2")
                pT = pps("pT2", [128, 1024], BF16); b_pT = B("pT2")
                pX = pps("pX", [128, 512]); b_pX = B("pX")
                for h in range(8):
                    S.op("pe", (lambda e, h=h: e.transpose(out=pT[:, h * 128:(h + 1) * 128], in_=wsb[:, h, :], identity=ident_bf[:, :])),
                         (b_c2, b_ident), (b_pT,))
                S.op("dve", lambda e: e.tensor_copy(out=WT[:, :, :], in_=pT[:, :].rearrange("p (h n) -> p h n", n=128)), (b_pT,), (b_c2,))
                u = psb("u", [128, AW]); va = psb("va", [128, AW]); vb = psb("vb", [128, BW]); r_ = psb("r", [128, BW])
                b_u = B("u"); b_va = B("va"); b_vb = B("vb"); b_r = B("r")
                q = [psb("q%d" % i, [128, BKW]) for i in range(2)]; kk = [psb("k%d" % i, [128, BKW]) for i in range(2)]
                gg = [psb("g%d" % i, [128, 16]) for i in range(2)]
                b_q = [B("q%d" % i) for i in range(2)]; b_k = [B("k%d" % i) for i in range(2)]; b_g = [B("g%d" % i) for i in range(2)]
                tA = psb("tA", [128, AW]); b_tA = B("tA")
                vn = psb("vn", [128, AW], BF16); b_vn = B("vn")
                yA = psb("yA", [128, AW]); b_yA = B("yA")
                yc = [psb("yc%d" % i, [128, D], BF16) for i in range(2)]; b_yc = [B("yc%d" % i) for i in range(2)]
                st2 = psb("st2", [128, 16]); b_st2 = B("st2"); b_st2b = B("st2b"); b_st2c = B("st2c")
                esb = psb("esb", [128, BKW]); b_esb = B("esb")
                la = psb("la", [128, BKW]); b_la = B("la")
                E = [psb("E%d" % i, [128, BKW]) for i in range(2)]; b_E = [B("E%d" % i) for i in range(2)]
                qd = psb("qd", [128, BKW], BF16); kd = psb("kd", [128, BKW], BF16); ks = psb("ks", [128, BKW], BF16)
                b_qd = B("qd"); b_kd = B("kd"); b_ks = B("ks")
                qdT = psb("qdT", [128, 8, 128], BF16); kdT = psb("kdT", [128, 8, 128], BF16); b_qdT = B("qdT"); b_kdT = B("kdT")
                vbf = psb("vbf", [128, BW], BF16); b_vbf = B("vbf")
                sr = psb("sr", [128, BW]); b_sr = B("sr")
                dec = psb("dec", [128, 8]); b_dec = B("dec")
                att = [psb("att%d" % i, [128, 128], BF16) for i in range(2)]; b_att = [B("att%d" % i) for i in range(2)]
                tn = [psb("tn%d" % i, [128, 512]) for i in range(2)]; b_tn = [B("tn%d" % i) for i in range(2)]
                so = psb("so", [128, 8]); b_so = [B("so%d" % i) for i in range(4)]

                pmask = psb("pmask_sb", [128, NP])
                dma("sp", pmask[:, :], I("pmask")[:, :], (), (b_c2,))
                items = [("p", j) for j in range(NP)] + [("m", j) for j in range(nt)]

                def ld_A(ii):
                    kind, tj = items[ii]
                    if kind != "m":
                        return
                    rw = slice(tj * 128, tj * 128 + 128); gj = tj // G
                    dma("sp", u[:, :], pu[rw, :], (b_proj[gj],), (b_u,))
                    dma("sp", va[:, :], pva[rw, :], (b_proj[gj],), (b_va,))

                def ld_qkg(ii):
                    kind, tj = items[ii]
                    rw = slice(tj * 128, tj * 128 + 128); gj = tj // G; sj = ii % 2
                    if kind == "m":
                        dma("sp", gg[sj][:, :], pg[rw, :], (b_proj[gj],), (b_g[sj],))
                        dma("sp", q[sj][:, :], pq[rw, :], (b_proj[gj],), (b_q[sj],))
                        dma("sp", kk[sj][:, :], pk[rw, :], (b_proj[gj],), (b_k[sj],))
                    else:
                        dma("sp", gg[sj][:, :], ppg[rw, :], (b_pp,), (b_g[sj],))
                        dma("sp", kk[sj][:, :], ppk[rw, :], (b_pp,), (b_k[sj],))

                def ld_B(ii):
                    kind, tj = items[ii]
                    rw = slice(tj * 128, tj * 128 + 128); gj = tj // G
                    if kind == "m":
                        dma("sp", vb[:, :], pvb[rw, :], (b_proj[gj],), (b_vb,))
                        dma("sp", r_[:, :], pr[rw, :], (b_proj[gj],), (b_r,))
                    else:
                        dma("sp", vb[:, :], ppv[rw, :], (b_pp,), (b_vb,))

                nit = len(items)
                for ii, (kind, ti) in enumerate(items):
                    so_ = (kind == "p")
                    g = ti // G
                    r0 = ti * 128
                    s_ = ii % 2
                    rows = slice(r0, r0 + 128)
                    if ii == 0:
                        ld_A(0); ld_qkg(0); ld_B(0)
                    if ii + 1 < nit:
                        ld_qkg(ii + 1)
                    if so_ and ii + 1 < nit and items[ii + 1][0] == "m":
                        ld_A(ii + 1)
                    S.mute = so_
                    s1 = st2[:, 0:1]; s2 = st2[:, 1:2]; mean = st2[:, 2:3]; msq = st2[:, 3:4]; var = st2[:, 4:5]
                    rs = st2[:, 5:6]; nmr = st2[:, 6:7]; ssy = st2[:, 7:8]; rsy = st2[:, 8:9]
                    S.op("dve", lambda e: e.memset(st2[:, 0:2], 0.0), (), (b_st2,))
                    S.op("act", lambda e: e.activation(out=tA[:, :], in_=va[:, :], func=AF.Identity, accum_out=st2[:, 0:1]),
                         (b_va, b_c2), (b_tA, b_st2))
                    S.op("act", lambda e: e.activation(out=tA[:, :], in_=va[:, :], func=AF.Square, accum_out=st2[:, 1:2]),
                         (b_va,), (b_tA, b_st2))

                    S.op("dve", lambda e: e.tensor_scalar(out=st2[:, 2:3], in0=st2[:, 0:1], scalar1=1.0 / AW, scalar2=None, op0=ALU.mult),
                         (b_st2,), (b_st2,))
                    S.op("dve", lambda e: e.tensor_tensor(out=st2[:, 3:4], in0=st2[:, 2:3], in1=st2[:, 2:3], op=ALU.mult), (b_st2,), (b_st2,))
                    S.op("dve", lambda e: e.scalar_tensor_tensor(out=st2[:, 4:5], in0=st2[:, 1:2], scalar=1.0 / AW, in1=st2[:, 3:4],
                                                                 op0=ALU.mult, op1=ALU.subtract), (b_st2,), (b_st2,))
                    rstd(st2[:, 5:6], st2[:, 4:5], 1.0, (b_st2,))
                    S.op("dve", lambda e: e.scalar_tensor_tensor(out=st2[:, 6:7], in0=st2[:, 2:3], scalar=-1.0, in1=st2[:, 5:6],
                                                                 op0=ALU.mult, op1=ALU.mult), (b_st2,), (b_st2,))
                    S.op("act", lambda e: e.activation(out=tA[:, :], in_=va[:, :], func=AF.Identity, bias=st2[:, 6:7], scale=st2[:, 5:6]),
                         (b_va, b_st2), (b_tA,))
                    S.op("dve", lambda e: e.tensor_tensor(out=tA[:, :], in0=tA[:, :], in1=lng[:, :], op=ALU.mult), (b_tA, b_c2), (b_tA,))
                    S.op("dve", lambda e: e.tensor_tensor(out=vn[:, :], in0=tA[:, :], in1=lnb[:, :], op=ALU.add), (b_tA, b_c2), (b_vn,))
                    for h in range(8):
                        p_ = (h // 2) % 2
                        o_ = (h % 2) * 256
                        S.op("pe", (lambda e, h=h, p_=p_, o_=o_: e.matmul(pA[p_][:, o_:o_ + 256], lhsT=WT[:, h, :], rhs=vn[:, h * 256:(h + 1) * 256],
                                                                        start=True, stop=True)), (b_vn, b_c2), (b_pA[p_],))
                        S.op("dve", (lambda e, h=h, p_=p_, o_=o_: e.scalar_tensor_tensor(out=yA[:, h * 256:(h + 1) * 256], in0=pA[p_][:, o_:o_ + 256],
                                                                                       scalar=bsT[:, h:h + 1], in1=u[:, h * 256:(h + 1) * 256],
                                                                                       op0=ALU.add, op1=ALU.mult)),
                             (b_pA[p_], b_u, b_c2), (b_yA,))
                    if ii + 1 < nit:
                        ld_A(ii + 1)
                    S.op("dve", lambda e: e.memset(st2[:, 7:8], 0.0), (), (b_st2b,))
                    S.op("act", lambda e: e.activation(out=tA[:, :], in_=yA[:, :], func=AF.Square, accum_out=st2[:, 7:8]),
                         (b_yA,), (b_tA, b_st2b))
                    rstd(st2[:, 8:9], st2[:, 7:8], AW, (b_st2b,))
                    S.op("dve", (lambda e, s_=s_: e.scalar_tensor_tensor(out=yc[s_][:, 0:AW], in0=yA[:, :], scalar=st2[:, 8:9], in1=aog[:, :],
                                                                        op0=ALU.mult, op1=ALU.mult)), (b_yA, b_st2b, b_c2), (b_yc[s_],), hard=True)
                    S.mute = False
                    S.op("pe", (lambda e, s_=s_: e.transpose(out=pX[0:16, 0:128], in_=gg[s_][:, :], identity=ident_f[:, :])),
                         (b_g[s_], b_ident), (b_pX,))
                    S.op("dve", lambda e: e.tensor_copy(out=glT[0:16, :], in_=pX[0:16, 0:128]), (b_pX, b_c2), (b_c2,))
                    for hf in range(2):
                        S.op("pe", (lambda e, hf=hf: e.matmul(P1[:, hf * 512:(hf + 1) * 512], lhsT=glT[0:17, :], rhs=wg2[0:17, hf * 512:(hf + 1) * 512],
                                                              start=True, stop=True)), (b_c2,), (b_P1,))
                    S.op("act", lambda e: e.activation(out=esb[:, :], in_=P1[:, :], func=AF.Exp, scale=-1.0), (b_P1,), (b_esb,))
                    S.op("act", lambda e: e.activation(out=la[:, :], in_=esb[:, :], func=AF.Ln, bias=one_c[:, 0:1], scale=1.0), (b_esb, b_ident), (b_la,))
                    S.mute = so_
                    for hf in range(2):
                        S.op("pe", (lambda e, hf=hf: e.matmul(P2[:, hf * 512:(hf + 1) * 512], lhsT=Um[:, :], rhs=la[:, hf * 512:(hf + 1) * 512],
                                                              start=True, stop=True)), (b_la, b_c2), (b_P2,))
                    S.mute = False
                    for hf in range(2):
                        S.op("pe", (lambda e, hf=hf: e.matmul(P1[:, hf * 512:(hf + 1) * 512], lhsT=Lm[:, :], rhs=la[:, hf * 512:(hf + 1) * 512],
                                                              start=True, stop=True)), (b_la, b_c2), (b_P1,))
                    for c in range(8):
                        S.op("pe", (lambda e, c=c: e.matmul(pX[:, 256 + c:257 + c], lhsT=la[:, c * 128:(c + 1) * 128], rhs=negc[:, 0:1],
                                                            start=True, stop=True)), (b_la, b_c2), (b_pX,))
                    S.op("act", lambda e: e.activation(out=dec[:, :], in_=pX[:, 256:264], func=AF.Exp), (b_pX,), (b_dec,))
                    S.mute = so_
                    S.op("act", lambda e: e.activation(out=E[0][:, :], in_=P2[:, :], func=AF.Exp), (b_P2,), (b_E[0],))
                    S.op("dve", (lambda e, s_=s_: e.scalar_tensor_tensor(out=qd[:, :], in0=q[s_][:, :], scalar=0.0625, in1=E[0][:, :],
                                                                        op0=ALU.mult, op1=ALU.mult)), (b_q[s_], b_E[0]), (b_qd,))
                    S.op("act", lambda e: e.activation(out=E[1][:, :], in_=P2[:, :], func=AF.Exp, scale=-1.0), (b_P2,), (b_E[1],))
                    S.op("dve", (lambda e, s_=s_: e.tensor_tensor(out=kd[:, :], in0=kk[s_][:, :], in1=E[1][:, :], op=ALU.mult)),
                         (b_k[s_], b_E[1]), (b_kd,))
                    S.mute = False
                    S.op("act", lambda e: e.activation(out=E[0][:, :], in_=P1[:, :], func=AF.Exp), (b_P1,), (b_E[0],))
                    S.op("dve", (lambda e, s_=s_: e.tensor_tensor(out=ks[:, :], in0=kk[s_][:, :], in1=E[0][:, :], op=ALU.mult)),
                         (b_k[s_], b_E[0]), (b_ks,))
                    if so_:
                        S.op("act", (lambda e, ti=ti: e.activation(out=vbf[:, :], in_=vb[:, :], func=AF.Copy, scale=pmask[:, ti:ti + 1])),
                             (b_vb, b_c2), (b_vbf,))
                    else:
                        S.op("act", lambda e: e.activation(out=vbf[:, :], in_=vb[:, :], func=AF.Copy), (b_vb,), (b_vbf,))
                    S.mute = so_
                    S.op("act", lambda e: e.activation(out=sr[:, :], in_=r_[:, :], func=AF.Silu), (b_r,), (b_sr,))
                    S.mute = False
                    if ii + 1 < nit:
                        ld_B(ii + 1)
                    S.mute = so_
                    for (srcb, b_src, dstT, b_dst, eng) in ((qd, b_qd, qdT, b_qdT, "act"), (kd, b_kd, kdT, b_kdT, "dve")):
                        for c in range(8):
                            S.op("pe", (lambda e, c=c, srcb=srcb: e.transpose(out=pT[:, c * 128:(c + 1) * 128], in_=srcb[:, c * 128:(c + 1) * 128],
                                                                            identity=ident_bf[:, :])), (b_src, b_ident), (b_pT,))
                        if eng == "act":
                            S.op("act", (lambda e, dstT=dstT: e.activation(out=dstT[:, :, :], in_=pT[:, :].rearrange("p (h n) -> p h n", n=128), func=AF.Copy)),
                                 (b_pT,), (b_dst,))
                        else:
                            S.op("dve", (lambda e, dstT=dstT: e.tensor_copy(out=dstT[:, :, :], in_=pT[:, :].rearrange("p (h n) -> p h n", n=128))),
                                 (b_pT,), (b_dst,))
                    for h in range(4):
                        a_ = h % 2
                        S.mute = so_
                        for c in range(2):
                            S.op("pe", (lambda e, h=h, c=c: e.matmul(pX[:, 0:128], lhsT=kdT[:, 2 * h + c, :], rhs=qdT[:, 2 * h + c, :],
                                                                   start=(c == 0), stop=(c == 1))), (b_kdT, b_qdT), (b_pX,))
                        S.op("dve", (lambda e, a_=a_: e.tensor_tensor(out=att[a_][:, :], in0=pX[:, 0:128], in1=Mk[:, :], op=ALU.mult)),
                             (b_pX, b_c2), (b_att[a_],))
                        S.op("pe", (lambda e, h=h, a_=a_: e.matmul(pA[a_][:, :], lhsT=att[a_][:, :], rhs=vbf[:, h * 512:(h + 1) * 512],
                                                                 start=True, stop=False)), (b_att[a_], b_vbf), (b_pA[a_],))
                        for c in range(2):
                            S.op("pe", (lambda e, h=h, c=c, a_=a_: e.matmul(pA[a_][:, :], lhsT=qdT[:, 2 * h + c, :], rhs=Sb[:, 2 * h + c, :],
                                                                          start=False, stop=(c == 1))), (b_qdT, b_S), (b_pA[a_],))
                        S.mute = False
                        for c in range(2):
                            ch = 2 * h + c
                            S.op("pe", (lambda e, h=h, ch=ch, c=c: e.matmul(P2[:, c * 512:(c + 1) * 512], lhsT=ks[:, ch * 128:(ch + 1) * 128],
                                                                          rhs=vbf[:, h * 512:(h + 1) * 512], start=True, stop=True)),
                                 (b_ks, b_vbf), (b_P2,))
                            S.op("dve", (lambda e, ch=ch, c=c: e.scalar_tensor_tensor(out=Sf[:, ch, :], in0=Sf[:, ch, :], scalar=dec[:, ch:ch + 1],
                                                                                    in1=P2[:, c * 512:(c + 1) * 512], op0=ALU.mult, op1=ALU.add)),
                                 (b_P2, b_dec, b_S), (b_S,))
                            if (not so_) or ii == NP - 1:
                                S.op("act", (lambda e, ch=ch: e.activation(out=Sb[:, ch, :], in_=Sf[:, ch, :], func=AF.Copy)), (b_S,), (b_S,))
                        S.mute = so_
                        S.op("dve", (lambda e, h=h: e.memset(so[:, 2 * h:2 * h + 1], 0.0)), (), (b_so[h],))
                        S.op("act", (lambda e, h=h, a_=a_: e.activation(out=tn[a_][:, :], in_=pA[a_][:, :], func=AF.Square, accum_out=so[:, 2 * h:2 * h + 1])),
                             (b_pA[a_],), (b_tn[a_], b_so[h]))
                        rstd(so[:, 2 * h + 1:2 * h + 2], so[:, 2 * h:2 * h + 1], 512, (b_so[h],))
                        S.op("dve", (lambda e, h=h, a_=a_: e.scalar_tensor_tensor(out=tn[a_][:, :], in0=pA[a_][:, :], scalar=so[:, 2 * h + 1:2 * h + 2],
                                                                                in1=hgb[:, :], op0=ALU.mult, op1=ALU.mult)),
                             (b_pA[a_], b_so[h], b_c2), (b_tn[a_],), hard=True)
                        S.op("dve", (lambda e, h=h, a_=a_, s_=s_: e.tensor_tensor(out=yc[s_][:, AW + h * 512:AW + (h + 1) * 512], in0=tn[a_][:, :],
                                                                                 in1=sr[:, h * 512:(h + 1) * 512], op=ALU.mult)),
                             (b_tn[a_], b_sr), (b_yc[s_],))
                    if not so_:
                        dma("sp", ycat[rows, :], yc[s_][:, :], (b_yc[s_],), (b_ycat[g],))
                    S.mute = False
                    if ti == 0 and not so_:
                        dbg("st2", st2[:, :], b_st2b, [128, 16]); dbg("vn", vn[:, :], b_vn, [128, AW], BF16)
                        dbg("yA", yA[:, :], b_yA, [128, AW]); dbg("la", la[:, :], b_la, [128, BKW])
                        dbg("qd", qd[:, :], b_qd, [128, BKW], BF16); dbg("kd", kd[:, :], b_kd, [128, BKW], BF16)
                        dbg("ks", ks[:, :], b_ks, [128, BKW], BF16); dbg("dec", dec[:, :], b_dec, [128, 8])
                        dbg("qdT", qdT[:, :, :], b_qdT, [128, 8, 128], BF16); dbg("att1", att[1][:, :], b_att[1], [128, 128], BF16)
                        dbg("tn1", tn[1][:, :], b_tn[1], [128, 512]); dbg("so", so[:, :], b_so[3], [128, 8])
                        dbg("WT", WT[:, :, :], b_c2, [128, 8, 128], BF16); dbg("Um", Um[:, :], b_c2, [128, 128])
                        dbg("Lm", Lm[:, :], b_c2, [128, 128]); dbg("Mk", Mk[:, :], b_c2, [128, 128])
                        dbg("glT", glT[:, :], b_c2, [32, 128]); dbg("Sf", Sf[:, :, :], b_S, [128, 8, 512])
                        dbg("sr", sr[:, :], b_sr, [128, BW]); dbg("vbf", vbf[:, :], b_vbf, [128, BW], BF16)
                        dbg("lng", lng[:, :], b_c2, [128, AW])
            finals += b_ycat

        b_yraw = [B("yraw%d" % g) for g in range(NG)]
        if phases >= 3:
            S.barrier()
            with ExitStack() as ph:
                psb = lambda name, shape, dt=F32: ph.enter_context(nc.sbuf_tensor(name, list(shape), dt))
                yT = psb("yT3", [128, NK, GT], BF16)
                yl = [psb("yl%d" % i, [128, D], BF16) for i in range(2)]
                b_yl = [B("yl%d" % i) for i in range(2)]
                psT = [ph.enter_context(nc.psum_tensor("psT3_%d" % i, [128, 1024], BF16)) for i in range(2)]
                b_psT = [B("psT3_%d" % i) for i in range(2)]
                w_out_v = I("w_out").rearrange("(k p) n -> p k n", p=128)
                gemm3 = make_gemm(ph, psb, "g3")
                for g in range(NG):
                    b_yT = [B("yT3_%d_%d" % (g, t)) for t in range(G)]
                    for t in range(G):
                        s_ = t % 2
                        r0 = (g * G + t) * 128
                        dma("sp", yl[s_][:, :], ycat[r0:r0 + 128, :], (b_ycat[g],), (b_yl[s_],))
                        aprep_transposes(psT, b_psT, yl[s_], b_yl[s_], yT, b_yT[t], t)

                    def store3(t, bi, stage, b_stage, g=g):
                        r0 = (g * G + t) * 128
                        dma("sp", yraw[r0:r0 + 128, bi * 512:(bi + 1) * 512], stage[:, :], (b_stage,), (b_yraw[g],))
                    gemm3(yT, b_yT, G, w_out_v, [(n * 512, 512) for n in range(D // 512)], store3)
            finals += b_yraw

        def load_gate_bc(psb, dst, tmp, col0, gname, bbuf, add_one=False):
            dma("sp", dst[:, :], modrow[0:1, col0:col0 + D].to_broadcast([128, D]), (b_modrow,), (bbuf,))
            if add_one:
                S.op("dve", lambda e: e.tensor_scalar(out=dst[:, :], in0=dst[:, :], scalar1=1.0, scalar2=None, op0=ALU.add), (bbuf,), (bbuf,))
            bt = B("gbc_tmp")
            dma("sp", tmp[:, :], I(gname)[0:1, :].to_broadcast([128, D]), (), (bt,))
            S.op("pool", lambda e: e.tensor_tensor(out=dst[:, :], in0=dst[:, :], in1=tmp[:, :], op=ALU.mult), (bbuf, bt), (bbuf,))
            return bt

        NSLOT = NE * CAP
        slot_tok = dscr("slot_tok", [NSLOT, 1], I32)
        b_x1 = B("x1"); b_xn2 = B("xn2"); b_slot_tok = B("slot_tok")
        rt_keep = None
        if phases >= 4:
            S.barrier()
            ph4 = top
            slot_i = sb("slot_i", [128, nt, 8], I32)
            w_all = sb("w_all", [128, nt, 8])
            b_tab = B("tab")
            with ExitStack() as ph:
                psb = lambda name, shape, dt=F32: ph.enter_context(nc.sbuf_tensor(name, list(shape), dt))
                pps = lambda name, shape, dt=F32: ph.enter_context(nc.psum_tensor(name, list(shape), dt))
                A2, B2, b_mod2 = load_modT(psb, "m2", 3 * D, "gT_pre_ffn")
                G1 = psb("G1", [128, D]); b_G1 = B("G1")
                yr = [psb("yr%d" % i, [128, D]) for i in range(2)]; b_yr = [B("yr%d" % i) for i in range(2)]
                xx = [psb("xx%d" % i, [128, D]) for i in range(2)]; b_xx = [B("xx%d" % i) for i in range(2)]
                xnb = psb("xnb", [128, D], BF16); b_xnb = B("xnb")
                h2Tf = psb("h2Tf", [128, NK, 128]); b_h2Tf = B("h2Tf")
                load_gate_bc(psb, G1, yr[1], 2 * D, "g_post_mix", b_G1)
                b_yr[1].W = list(b_G1.W); b_yr[1].R = list(b_G1.R)
                A2bc = psb("A2bc", [128, D]); B2bc = psb("B2bc", [128, D]); b_m2bc = B("m2bc")
                load_gate_bc(psb, A2bc, yr[0], 4 * D, "g_pre_ffn", b_m2bc, add_one=True)
                b_yr[0].W = list(b_m2bc.W); b_yr[0].R = list(b_m2bc.R)
                dma("sp", B2bc[:, :], modrow[0:1, 3 * D:4 * D].to_broadcast([128, D]), (b_modrow,), (b_m2bc,))
                wr = psb("wr", [128, NK, NE]); b_c4 = B("c4")
                dma("sp", wr[:, :, :], I("w_router").rearrange("(k p) e -> p k e", p=128), (), (b_c4,))
                rb = psb("rb", [128, NE])
                dma("sp", rb[:, :], I("router_bias")[0:1, :].to_broadcast([128, NE]), (), (b_c4,))
                SLT = psb("SLT", [128, 128], BF16); ONES = psb("ONES", [128, 128], BF16)
                tmpf = psb("tmpf", [128, 128])
                S.op("pool", lambda e: e.memset(tmpf[:, :], 1.0), (), (b_c4,))
                S.op("pool", lambda e: e.affine_select(out=tmpf[:, :], in_=tmpf[:, :], pattern=[[1, 128]], compare_op=ALU.is_gt,
                                                       fill=0.0, base=0, channel_multiplier=-1), (b_c4,), (b_c4,))
                S.op("pool", lambda e: e.tensor_copy(out=SLT[:, :], in_=tmpf[:, :]), (b_c4,), (b_c4,))
                S.op("pool", lambda e: e.memset(ONES[:, :], 1.0), (), (b_c4,))
                eoff = psb("eoff", [128, NE])
                S.op("pool", lambda e: e.iota(eoff[:, :], [[CAP, NE]], base=0, channel_multiplier=0,
                                              allow_small_or_imprecise_dtypes=True), (), (b_c4,))
                tokid = psb("tokid", [128, nt], I32)
                S.op("pool", lambda e: e.iota(tokid[:, :], [[128, nt]], base=0, channel_multiplier=1), (), (b_c4,))
                cntb = psb("cntb", [128, NE]); b_cnt = B("cntb")
                S.op("pool", lambda e: e.memset(cntb[:, :], 0.0), (), (b_cnt,))
                zt = psb("zt", [128, NSLOT // 128], I32)
                S.op("pool", lambda e: e.memset(zt[:, :], 0), (), (b_c4,))
                dma("sp", slot_tok.rearrange("(p n) o -> p (n o)", p=128), zt[:, :], (b_c4,), (b_slot_tok,))
                st4 = psb("st4", [128, 8]); b_st4 = [B("st4_%d" % i) for i in range(2)]
                sc = psb("sc", [128, NE]); sel = psb("sel", [128, NE]); tmp64 = psb("tmp64", [128, NE]); eq = psb("eq", [128, NE])
                msel = psb("msel", [128, NE]); Mm = psb("Mm", [128, NE]); Mb = psb("Mb", [128, NE], BF16); Wt = psb("Wt", [128, NE])
                pos = psb("pos", [128, NE]); okM = psb("okM", [128, NE]); slotf = psb("slotf", [128, NE])
                sm = psb("sm", [128, 64])
                scat_f = psb("scat_f", [128, 8]); scat_i = psb("scat_i", [128, 8], I32)
                slot_f = psb("slot_f", [128, 8])
                b_rt = B("rt"); b_scat = B("scat")
                psTf = [pps("psTf%d" % i, [128, 512]) for i in range(2)]; b_psTf = [B("psTf%d" % i) for i in range(2)]
                psR = pps("psR", [128, NE]); b_psR = B("psR")
                psP = pps("psP", [128, 128]); b_psP = B("psP")
                BIG = 1.0e9
                for ti in range(nt):
                    g = ti // G
                    s_ = ti % 2
                    rows = slice(ti * 128, ti * 128 + 128)
                    dma("sp", yr[s_][:, :], yraw[rows, :], (b_yraw[g],), (b_yr[s_],))
                    dma("sp", xx[s_][:, :], I("x")[rows, :], (), (b_xx[s_],))
                    ssv = st4[:, 4 * s_:4 * s_ + 1]; rsv = st4[:, 4 * s_ + 1:4 * s_ + 2]
                    ss2 = st4[:, 4 * s_ + 2:4 * s_ + 3]; rs2 = st4[:, 4 * s_ + 3:4 * s_ + 4]
                    S.op("dve", (lambda e, s_=s_: e.memset(st4[:, 4 * s_:4 * s_ + 4], 0.0)), (), (b_st4[s_],))
                    S.op("act", (lambda e, s_=s_, ssv=ssv: e.activation(out=xnb[:, :], in_=yr[s_][:, :], func=AF.Square, accum_out=ssv)),
                         (b_yr[s_],), (b_xnb, b_st4[s_]))
                    rstd(rsv, ssv, D, (b_st4[s_],))
                    S.op("dve", (lambda e, s_=s_, rsv=rsv: e.scalar_tensor_tensor(out=yr[s_][:, :], in0=yr[s_][:, :], scalar=rsv, in1=G1[:, :],
                                                                                 op0=ALU.mult, op1=ALU.mult)), (b_yr[s_], b_st4[s_], b_G1), (b_yr[s_],))
                    S.op("dve", (lambda e, s_=s_: e.tensor_tensor(out=xx[s_][:, :], in0=xx[s_][:, :], in1=yr[s_][:, :], op=ALU.add)),
                         (b_xx[s_], b_yr[s_]), (b_xx[s_],))
                    dma("sp", x1[rows, :], xx[s_][:, :], (b_xx[s_],), (b_x1,))
                    S.op("act", (lambda e, s_=s_, ss2=ss2: e.activation(out=xnb[:, :], in_=xx[s_][:, :], func=AF.Square, accum_out=ss2)),
                         (b_xx[s_],), (b_xnb, b_st4[s_]))
                    rstd(rs2, ss2, D, (b_st4[s_],))
                    S.op("act", (lambda e, s_=s_, rs2=rs2: e.activation(out=yr[s_][:, :], in_=xx[s_][:, :], func=AF.Copy, scale=rs2)),
                         (b_xx[s_], b_st4[s_]), (b_yr[s_],))
                    S.op("dve", (lambda e, s_=s_: e.tensor_tensor(out=xx[s_][:, :], in0=yr[s_][:, :], in1=A2bc[:, :], op=ALU.mult)),
                         (b_yr[s_], b_m2bc), (b_xx[s_],))
                    S.op("dve", (lambda e, s_=s_: e.tensor_tensor(out=xnb[:, :], in0=xx[s_][:, :], in1=B2bc[:, :], op=ALU.add)),
                         (b_xx[s_], b_m2bc), (b_xnb,))
                    dma("sp", xn2[rows, :], xnb[:, :], (b_xnb,), (b_xn2,))
                    for c4 in range(NK // 4):
                        p_ = c4 % 2
                        for j in range(4):
                            c = c4 * 4 + j
                            S.op("pe", (lambda e, p_=p_, j=j, c=c, s_=s_: e.transpose(out=psTf[p_][:, j * 128:(j + 1) * 128],
                                                                                    in_=yr[s_][:, c * 128:(c + 1) * 128], identity=ident_f[:, :])),
                                 (b_yr[s_], b_ident), (b_psTf[p_],))
                        for j in range(4):
                            c = c4 * 4 + j
                            if j % 2 == 0:
                                S.op("act", (lambda e, p_=p_, j=j, c=c: e.activation(out=h2Tf[:, c, :], in_=psTf[p_][:, j * 128:(j + 1) * 128], func=AF.Identity,
                                                                                   bias=B2[:, c:c + 1], scale=A2[:, c:c + 1])), (b_psTf[p_], b_mod2), (b_h2Tf,))
                            else:
                                S.op("dve", (lambda e, p_=p_, j=j, c=c: e.tensor_scalar(out=h2Tf[:, c, :], in0=psTf[p_][:, j * 128:(j + 1) * 128], scalar1=A2[:, c:c + 1],
                                                                                      scalar2=B2[:, c:c + 1], op0=ALU.mult, op1=ALU.add)), (b_psTf[p_], b_mod2), (b_h2Tf,))
                    for k in range(NK):
                        S.op("pe", (lambda e, k=k: e.matmul(psR[:, :], lhsT=h2Tf[:, k, :], rhs=wr[:, k, :], start=(k == 0), stop=(k == NK - 1))),
                             (b_h2Tf, b_c4), (b_psR,))
                    R_ = lambda fn, extra_r=(), extra_w=(): S.op("dve", fn, (b_rt, b_c4) + tuple(extra_r), (b_rt,) + tuple(extra_w))
                    S.op("act", lambda e: e.activation(out=sc[:, :], in_=psR[:, :], func=AF.Sigmoid), (b_psR,), (b_rt,))
                    R_(lambda e: e.tensor_tensor(out=sel[:, :], in0=sc[:, :], in1=rb[:, :], op=ALU.add))
                    v3 = lambda t_: t_[:, :].rearrange("p (g e) -> p g e", e=8)
                    bc3 = lambda col: col.unsqueeze(2).to_broadcast([128, 8, 8])
                    R_(lambda e: e.tensor_reduce(out=sm[:, 0:8], in_=v3(sel), axis=AX.X, op=ALU.max))
                    R_(lambda e: e.tensor_tensor(out=v3(eq), in0=v3(sel), in1=bc3(sm[:, 0:8]), op=ALU.is_equal))
                    R_(lambda e: e.scalar_tensor_tensor(out=tmp64[:, :], in0=eq[:, :], scalar=-BIG, in1=sel[:, :], op0=ALU.mult, op1=ALU.add))
                    R_(lambda e: e.tensor_reduce(out=sm[:, 8:16], in_=v3(tmp64), axis=AX.X, op=ALU.max))
                    R_(lambda e: e.tensor_tensor(out=sm[:, 16:24], in0=sm[:, 0:8], in1=sm[:, 8:16], op=ALU.add))
                    R_(lambda e: e.max(out=sm[:, 24:32], in_=sm[:, 16:24]))
                    R_(lambda e: e.tensor_scalar(out=sm[:, 32:40], in0=sm[:, 16:24], scalar1=sm[:, 27:28], scalar2=None, op0=ALU.is_ge))
                    R_(lambda e: e.tensor_scalar(out=sm[:, 40:48], in0=sm[:, 32:40], scalar1=-1.0, scalar2=BIG, op0=ALU.add, op1=ALU.mult))
                    R_(lambda e: e.tensor_tensor(out=v3(msel), in0=v3(sel), in1=bc3(sm[:, 32:40]), op=ALU.mult))
                    R_(lambda e: e.tensor_tensor(out=v3(msel), in0=v3(msel), in1=bc3(sm[:, 40:48]), op=ALU.add))
                    R_(lambda e: e.max(out=sm[:, 48:56], in_=msel[:, :]))
                    R_(lambda e: e.tensor_scalar(out=Mm[:, :], in0=msel[:, :], scalar1=sm[:, 53:54], scalar2=None, op0=ALU.is_ge))
                    R_(lambda e: e.tensor_tensor(out=tmp64[:, :], in0=sc[:, :], in1=Mm[:, :], op=ALU.mult))
                    R_(lambda e: e.tensor_reduce(out=sm[:, 56:57], in_=tmp64[:, :], axis=AX.X, op=ALU.add))
                    R_(lambda e: e.reciprocal(out=sm[:, 57:58], in_=sm[:, 56:57]))
                    R_(lambda e: e.tensor_scalar(out=Wt[:, :], in0=tmp64[:, :], scalar1=sm[:, 57:58], scalar2=2.5, op0=ALU.mult, op1=ALU.mult))
                    R_(lambda e: e.tensor_copy(out=Mb[:, :], in_=Mm[:, :]))
                    S.op("pe", lambda e: e.matmul(psP[:, 0:NE], lhsT=SLT[:, :], rhs=Mb[:, :], start=True, stop=True), (b_rt, b_c4), (b_psP,))
                    S.op("pe", lambda e: e.matmul(psP[:, NE:2 * NE], lhsT=ONES[:, :], rhs=Mb[:, :], start=True, stop=True), (b_rt, b_c4), (b_psP,))
                    R_(lambda e: e.tensor_tensor(out=pos[:, :], in0=psP[:, 0:NE], in1=cntb[:, :], op=ALU.add), (b_psP, b_cnt))
                    S.op("dve", lambda e: e.tensor_tensor(out=cntb[:, :], in0=cntb[:, :], in1=psP[:, NE:2 * NE], op=ALU.add), (b_psP, b_cnt, b_rt), (b_cnt,))
                    R_(lambda e: e.tensor_scalar(out=okM[:, :], in0=pos[:, :], scalar1=float(CAP), scalar2=None, op0=ALU.is_lt))
                    R_(lambda e: e.tensor_tensor(out=okM[:, :], in0=okM[:, :], in1=Mm[:, :], op=ALU.mult))
                    R_(lambda e: e.tensor_tensor(out=slotf[:, :], in0=pos[:, :], in1=eoff[:, :], op=ALU.add))
                    R_(lambda e: e.tensor_tensor(out=slotf[:, :], in0=slotf[:, :], in1=okM[:, :], op=ALU.mult))
                    R_(lambda e: e.tensor_tensor(out=Wt[:, :], in0=Wt[:, :], in1=okM[:, :], op=ALU.mult))
                    R_(lambda e: e.memset(slot_f[:, :], 0.0))
                    R_(lambda e: e.memset(scat_f[:, :], 0.0), (), (b_scat,))
                    for k in range(6):
                        R_(lambda e, k=k: e.tensor_scalar(out=eq[:, :], in0=msel[:, :], scalar1=sm[:, 48 + k:49 + k], scalar2=None, op0=ALU.is_equal))
                        R_(lambda e: e.tensor_tensor(out=tmp64[:, :], in0=eq[:, :], in1=slotf[:, :], op=ALU.mult))
                        R_(lambda e, k=k: e.tensor_reduce(out=slot_f[:, k:k + 1], in_=tmp64[:, :], axis=AX.X, op=ALU.add))
                        R_(lambda e: e.tensor_tensor(out=tmp64[:, :], in0=eq[:, :], in1=Wt[:, :], op=ALU.mult))
                        R_(lambda e, k=k, ti=ti: e.tensor_reduce(out=w_all[:, ti, k:k + 1], in_=tmp64[:, :], axis=AX.X, op=ALU.add), (), (b_tab,))
                        R_(lambda e: e.tensor_tensor(out=tmp64[:, :], in0=eq[:, :], in1=okM[:, :], op=ALU.mult))
                        R_(lambda e, k=k: e.tensor_reduce(out=sm[:, 58 + k:59 + k], in_=tmp64[:, :], axis=AX.X, op=ALU.add))
                    R_(lambda e: e.tensor_scalar(out=scat_f[:, 0:6], in0=sm[:, 58:64], scalar1=-1.0, scalar2=-1.0e6, op0=ALU.add, op1=ALU.mult), (), (b_scat,))
                    R_(lambda e: e.tensor_tensor(out=scat_f[:, 0:6], in0=scat_f[:, 0:6], in1=slot_f[:, 0:6], op=ALU.add), (b_scat,), (b_scat,))
                    R_(lambda e: e.tensor_scalar(out=scat_f[:, :], in0=scat_f[:, :], scalar1=0.0, scalar2=2.0e6, op0=ALU.max, op1=ALU.min), (b_scat,), (b_scat,))
                    R_(lambda e: e.tensor_scalar(out=slot_f[:, :], in0=slot_f[:, :], scalar1=0.0, scalar2=float(NSLOT - 1), op0=ALU.max, op1=ALU.min))
                    R_(lambda e: e.tensor_copy(out=scat_i[:, :], in_=scat_f[:, :]), (b_scat,), (b_scat,))
                    R_(lambda e, ti=ti: e.tensor_copy(out=slot_i[:, ti, :], in_=slot_f[:, :]), (), (b_tab,))
                    for k in range(6 if not os.environ.get("NOSCAT") else 0):
                        def scat_fn(e, k=k, ti=ti):
                            if "bc" not in regcache:
                                regcache["bc"] = e.to_reg(NSLOT - 1)
                            return e.indirect_dma_start(
                                out=slot_tok[:, :], out_offset=bass.IndirectOffsetOnAxis(ap=scat_i[:, k:k + 1], axis=0),
                                in_=tokid[:, ti:ti + 1], in_offset=None, bounds_check=regcache["bc"], oob_is_err=False)
                        S.op("pool", scat_fn,
                            (b_scat, b_c4), (b_slot_tok,), dma=True, waw=(ti == 0 and k == 0))
                    if ti == 0:
                        dbg("Mm", Mm[:, :], b_rt, [128, NE]); dbg("Wt", Wt[:, :], b_rt, [128, NE]); dbg("sc", sc[:, :], b_rt, [128, NE])
                        dbg("sm", sm[:, :], b_rt, [128, 64]); dbg("pos", pos[:, :], b_rt, [128, NE])
                dbg("slot_i", slot_i[:, :, :], b_tab, [128, nt, 8], I32); dbg("w_all", w_all[:, :, :], b_tab, [128, nt, 8])
            finals += [b_x1, b_xn2, b_slot_tok]

        ybh = [nc.dram_tensor("yb%d" % i, [NSLOT, D // 2], F32, kind="Internal").ap() for i in range(2)]
        b_ysh = B("ysh"); b_yb = B("yb")
        if phases >= 5:
            S.barrier()
            with ExitStack() as ph:
                psb = lambda name, shape, dt=F32: ph.enter_context(nc.sbuf_tensor(name, list(shape), dt))
                pps = lambda name, shape, dt=F32: ph.enter_context(nc.psum_tensor(name, list(shape), dt))
                gath = [psb("gath%d" % i, [128, D], BF16) for i in range(2)]; b_gath = [B("gath%d" % i) for i in range(2)]
                idxs = [psb("idxs%d" % i, [128, 4], I32) for i in range(2)]; b_idx = [B("idxs%d" % i) for i in range(2)]
                xT = [psb("xT%d" % i, [128, NK, 512], BF16) for i in range(2)]; b_xT = [B("xT%d" % i) for i in range(2)]
                NWG = 4
                WgS = [psb("WgS%d" % i, [128, NK, 256], BF16) for i in range(NWG)]; b_Wg = [B("WgS%d" % i) for i in range(NWG)]
                NWD = 3
                WdS = [psb("WdS%d" % i, [128, NF, 512], BF16) for i in range(NWD)]; b_Wd = [B("WdS%d" % i) for i in range(NWD)]
                hidT = [psb("hidT%d" % i, [128, NF, 512], BF16) for i in range(2)]; b_hid = [B("hidT%d" % i) for i in range(2)]
                sg = [psb("sg%d" % i, [128, 512]) for i in range(2)]; b_sg = [B("sg%d" % i) for i in range(2)]
                stg = [psb("stg5_%d" % i, [128, 512]) for i in range(4)]; b_stg = [B("stg5_%d" % i) for i in range(4)]
                psT = [pps("psT5_%d" % i, [128, 1024], BF16) for i in range(2)]; b_psT = [B("psT5_%d" % i) for i in range(2)]
                pg_ = [pps("pg%d" % i, [128, 512]) for i in range(2)]; b_pg = [B("pg%d" % i) for i in range(2)]
                pu_ = [pps("pu%d" % i, [128, 512]) for i in range(2)]; b_pu = [B("pu%d" % i) for i in range(2)]
                po_ = [pps("po%d" % i, [128, 512]) for i in range(2)]; b_po = [B("po%d" % i) for i in range(2)]
                SC = min(512, T)
                items = [("s", c) for c in range(T // SC)] + [("r", e) for e in range(NE)]
                if os.environ.get("NEXP"):
                    items = items[:T // SC + int(os.environ["NEXP"])]
                cn = {"g": 0, "wg": 0, "wd": 0, "st": 0, "f": 0, "o": 0}
                def item_ctx(it):
                    kind, e = items[it]
                    xs_ = it % 2
                    ncols = SC if kind == "s" else CAP
                    nblk = ncols // 128
                    hs = it % 2
                    return kind, e, xs_, ncols, nblk, hs

                def wviews(kind, e):
                    if kind == "s":
                        wg_v = I("ws_gate").rearrange("(k p) f -> p k f", p=128)
                        wu_v = I("ws_up").rearrange("(k p) f -> p k f", p=128)
                        wd_v = I("ws_down").rearrange("(f p) n -> p f n", p=128)
                    else:
                        wg_v = I("we_gate")[e].rearrange("(k p) f -> p k f", p=128)
                        wu_v = I("we_up")[e].rearrange("(k p) f -> p k f", p=128)
                        wd_v = I("we_down")[e].rearrange("(f p) n -> p f n", p=128)
                    return wg_v, wu_v, wd_v

                def stageA(it):
                    kind, e, xs_, ncols, nblk, hs = item_ctx(it)
                    for blk in range(nblk):
                        gs = cn["g"] % 2
                        cn["g"] += 1
                        if kind == "s":
                            r0 = e * SC + blk * 128
                            dma("sp", gath[gs][:, :], xn2[r0:r0 + 128, :], (b_xn2,), (b_gath[gs],))
                        else:
                            r0 = e * CAP + blk * 128
                            dma("sp", idxs[gs][:, 0:1], slot_tok[r0:r0 + 128, 0:1], (b_slot_tok,), (b_idx[gs],))
                            S.op("pool", (lambda ee, gs=gs: ee.indirect_dma_start(
                                out=gath[gs][:, :], out_offset=None, in_=xn2[:, :],
                                in_offset=bass.IndirectOffsetOnAxis(ap=idxs[gs][:, 0:1], axis=0))),
                                (b_idx[gs], b_xn2), (b_gath[gs],), dma=True)
                        aprep_transposes(psT, b_psT, gath[gs], b_gath[gs], xT[xs_], b_xT[xs_], blk)

                def stageB(it):
                    kind, e, xs_, ncols, nblk, hs = item_ctx(it)
                    wg_v, wu_v, wd_v = wviews(kind, e)
                    for fp in range(NF // 2):
                        wgs = cn["wg"] % NWG; cn["wg"] += 1
                        wus = cn["wg"] % NWG; cn["wg"] += 1
                        dma("pool", WgS[wgs][:, :, :], wg_v[:, :, fp * 256:(fp + 1) * 256], (), (b_Wg[wgs],))
                        dma("pool", WgS[wus][:, :, :], wu_v[:, :, fp * 256:(fp + 1) * 256], (), (b_Wg[wus],))
                        for fc in range(2):
                            f = fp * 2 + fc
                            p_ = cn["f"] % 2; cn["f"] += 1
                            for k in range(NK):
                                S.op("pe", (lambda ee, p_=p_, wgs=wgs, k=k, fc=fc, xs_=xs_, ncols=ncols: ee.matmul(
                                    pg_[p_][:, 0:ncols], lhsT=WgS[wgs][:, k, fc * 128:(fc + 1) * 128], rhs=xT[xs_][:, k, 0:ncols],
                                    start=(k == 0), stop=(k == NK - 1))), (b_Wg[wgs], b_xT[xs_]), (b_pg[p_],))
                            for k in range(NK):
                                S.op("pe", (lambda ee, p_=p_, wus=wus, k=k, fc=fc, xs_=xs_, ncols=ncols: ee.matmul(
                                    pu_[p_][:, 0:ncols], lhsT=WgS[wus][:, k, fc * 128:(fc + 1) * 128], rhs=xT[xs_][:, k, 0:ncols],
                                    start=(k == 0), stop=(k == NK - 1))), (b_Wg[wus], b_xT[xs_]), (b_pu[p_],))
                            S.op("act", (lambda ee, p_=p_, ncols=ncols: ee.activation(out=sg[p_][:, 0:ncols], in_=pg_[p_][:, 0:ncols], func=AF.Silu)),
                                 (b_pg[p_],), (b_sg[p_],))
                            S.op("dve", (lambda ee, p_=p_, ncols=ncols, hs=hs, f=f: ee.tensor_tensor(out=hidT[hs][:, f, 0:ncols], in0=sg[p_][:, 0:ncols],
                                                                                                in1=pu_[p_][:, 0:ncols], op=ALU.mult)),
                                 (b_sg[p_], b_pu[p_]), (b_hid[hs],))

                def stageC(it):
                    kind, e, xs_, ncols, nblk, hs = item_ctx(it)
                    wg_v, wu_v, wd_v = wviews(kind, e)
                    for n in range(D // 512):
                        wds = cn["wd"] % NWD; cn["wd"] += 1
                        dma("pool", WdS[wds][:, :, :], wd_v[:, :, n * 512:(n + 1) * 512], (), (b_Wd[wds],))
                        for blk in range(nblk):
                            o_ = cn["o"] % 2; cn["o"] += 1
                            q_ = cn["st"] % 4; cn["st"] += 1
                            for f in range(NF):
                                S.op("pe", (lambda ee, o_=o_, hs=hs, f=f, blk=blk, wds=wds: ee.matmul(
                                    po_[o_][:, :], lhsT=hidT[hs][:, f, blk * 128:(blk + 1) * 128], rhs=WdS[wds][:, f, :],
                                    start=(f == 0), stop=(f == NF - 1))), (b_hid[hs], b_Wd[wds]), (b_po[o_],))
                            if cn["o"] % 2 == 0:
                                S.op("act", (lambda ee, o_=o_, q_=q_: ee.activation(out=stg[q_][:, :], in_=po_[o_][:, :], func=AF.Copy)), (b_po[o_],), (b_stg[q_],))
                            else:
                                S.op("dve", (lambda ee, o_=o_, q_=q_: ee.tensor_copy(out=stg[q_][:, :], in_=po_[o_][:, :])), (b_po[o_],), (b_stg[q_],))
                            if kind == "s":
                                r0 = e * SC + blk * 128
                                dma("sp", ysh[r0:r0 + 128, n * 512:(n + 1) * 512], stg[q_][:, :], (b_stg[q_],), (b_ysh,))
                            else:
                                r0 = e * CAP + blk * 128
                                hh, nn = n // 4, n % 4
                                dma("sp", ybh[hh][r0:r0 + 128, nn * 512:(nn + 1) * 512], stg[q_][:, :], (b_stg[q_],), (b_yb,))

                stageA(0)
                for it in range(len(items)):
                    stageB(it)
                    if it + 1 < len(items):
                        stageA(it + 1)
                    stageC(it)
            finals += [b_ysh, b_yb]

        b_out = B("out")
        if phases >= 6:
            S.barrier()
            with ExitStack() as ph:
                psb = lambda name, shape, dt=F32: ph.enter_context(nc.sbuf_tensor(name, list(shape), dt))
                G2 = psb("G2", [128, D]); b_G2 = B("G2")
                acc = [psb("acc%d" % i, [128, D]) for i in range(2)]; b_acc = [B("acc%d" % i) for i in range(2)]
                x1t = [psb("x1t%d" % i, [128, D]) for i in range(2)]; b_x1t = [B("x1t%d" % i) for i in range(2)]
                gk = [psb("gk%d" % i, [128, D]) for i in range(3)]; b_gk = [B("gk%d" % i) for i in range(3)]
                jk = psb("jk6", [128, D], BF16); b_jk = B("jk6")
                st6 = psb("st6", [128, 4]); b_st6 = [B("st6_%d" % i) for i in range(2)]
                bt = load_gate_bc(psb, G2, gk[0], 5 * D, "g_post_ffn", b_G2)
                b_gk[0].W = list(b_G2.W); b_gk[0].R = list(b_G2.R)
                ng = 0
                for ti in range(nt):
                    s_ = ti % 2
                    rows = slice(ti * 128, ti * 128 + 128)
                    dma("sp", acc[s_][:, :], ysh[rows, :], (b_ysh,), (b_acc[s_],))
                    dma("sp", x1t[s_][:, :], x1[rows, :], (b_x1,), (b_x1t[s_],))
                    for k in range(6):
                        gsl = ng % 3; ng += 1
                        for hh in range(2):
                            S.op("pool", (lambda ee, gsl=gsl, ti=ti, k=k, hh=hh: ee.indirect_dma_start(
                                out=gk[gsl][:, hh * (D // 2):(hh + 1) * (D // 2)], out_offset=None, in_=ybh[hh][:, :],
                                in_offset=bass.IndirectOffsetOnAxis(ap=slot_i[:, ti, k:k + 1], axis=0))),
                                (b_tab, b_yb), (b_gk[gsl],), dma=True)
                        S.op("dve", (lambda ee, gsl=gsl, ti=ti, k=k, s_=s_: ee.scalar_tensor_tensor(
                            out=acc[s_][:, :], in0=gk[gsl][:, :], scalar=w_all[:, ti, k:k + 1], in1=acc[s_][:, :], op0=ALU.mult, op1=ALU.add)),
                            (b_gk[gsl], b_tab, b_acc[s_]), (b_acc[s_],))
                    ssv = st6[:, 2 * s_:2 * s_ + 1]; rsv = st6[:, 2 * s_ + 1:2 * s_ + 2]
                    S.op("dve", (lambda ee, ssv=ssv: ee.memset(ssv, 0.0)), (), (b_st6[s_],))
                    S.op("act", (lambda ee, s_=s_, ssv=ssv: ee.activation(out=jk[:, :], in_=acc[s_][:, :], func=AF.Square, accum_out=ssv)),
                         (b_acc[s_],), (b_jk, b_st6[s_]))
                    rstd(rsv, ssv, D, (b_st6[s_],))
                    S.op("dve", (lambda ee, s_=s_, rsv=rsv: ee.scalar_tensor_tensor(out=acc[s_][:, :], in0=acc[s_][:, :], scalar=rsv, in1=G2[:, :],
                                                                                  op0=ALU.mult, op1=ALU.mult)), (b_acc[s_], b_st6[s_], b_G2), (b_acc[s_],))
                    S.op("dve", (lambda ee, s_=s_: ee.tensor_tensor(out=x1t[s_][:, :], in0=x1t[s_][:, :], in1=acc[s_][:, :], op=ALU.add)),
                         (b_x1t[s_], b_acc[s_]), (b_x1t[s_],))
                    dma("sp", out[rows, :], x1t[s_][:, :], (b_x1t[s_],), (b_out,))
            finals += [b_out]

        finals += dbg_list
        S.emit(final_bufs=finals)
    S.used_inputs = list(used_inputs.keys())
    return nc, S


def _prep_inputs(inp):
    f = lambda a: np.ascontiguousarray(np.asarray(a, dtype=np.float32))
    x = f(inp["x"])
    c = f(inp["c"])
    L = 0
    common = {
        "w_ada": f(inp["w_ada"][L]),
        "b_ada": f(inp["b_ada"][L]).reshape(1, -1),
        "gT_pre_mix": f(inp["g_pre_mix"][L].reshape(NK, 128).T),
        "gT_pre_ffn": f(inp["g_pre_ffn"][L].reshape(NK, 128).T),
        "g_post_mix": f(inp["g_post_mix"][L]).reshape(1, -1),
        "g_pre_ffn": f(inp["g_pre_ffn"][L]).reshape(1, -1),
        "g_pre_mix": f(inp["g_pre_mix"][L]).reshape(1, -1),
        "g_post_ffn": f(inp["g_post_ffn"][L]).reshape(1, -1),
        "w_in": f(inp["w_in"][L]),
        "a_ln_g": f(inp["a_ln_g"][L]).reshape(1, -1),
        "a_ln_b": f(inp["a_ln_b"][L]).reshape(1, -1),
        "a_w_s": f(inp["a_w_s"][L]),
        "a_b_sT": f(inp["a_b_s"][L].T),
        "a_out_g": f(inp["a_out_g"][L]).reshape(1, -1),
        "b_w_g2a": f(np.concatenate([inp["b_w_g2"][L], inp["b_b_g2"][L].reshape(1, -1)], axis=0)),
        "b_head_g": f(inp["b_head_g"][L]).reshape(1, -1),
        "w_out": f(inp["w_out"][L]),
        "w_router": f(inp["w_router"][L]),
        "router_bias": f(inp["router_bias"][L]).reshape(1, -1),
        "we_gate": f(inp["we_gate"][L]),
        "we_up": f(inp["we_up"][L]),
        "we_down": f(inp["we_down"][L]),
        "ws_gate": f(inp["ws_gate"][L]),
        "ws_up": f(inp["ws_up"][L]),
        "ws_down": f(inp["ws_down"][L]),
    }
    return x, c, common


def prefix_inputs(xb, q, nt):
    T = nt * 128
    NP = 3 * nt
    xp = np.zeros((NP * 128, D), np.float32)
    pm = np.zeros((128, NP), np.float32)
    n = q * T
    if n:
        xp[NP * 128 - n:] = xb[0:n]
        pm[:, NP - n // 128:] = 1.0
    return xp, pm


def kernel(**inp):
    x, c, common = _prep_inputs(inp)
    nc, S = build(nt=16)
    in_maps = []
    for core in range(NCORES):
        b, q = core // 4, core % 4
        m = dict(common)
        m["x"] = np.ascontiguousarray(x[b, q * 2048:(q + 1) * 2048, :])
        m["cT"] = np.ascontiguousarray(c[b].reshape(NK, 128).T)
        xp, pm = prefix_inputs(x[b], q, 16)
        m["xprev"] = xp
        m["pmask"] = pm
        in_maps.append({k: m[k] for k in S.used_inputs})
    res = run_bass_kernel_spmd(nc, in_maps, core_ids=list(range(NCORES)))
    outs = [r["out"] for r in res.results]
    full = np.stack([np.concatenate(outs[0:4], axis=0), np.concatenate(outs[4:8], axis=0)], axis=0)
    return full.astype(np.float32)
```
